# Optimizing a Trainium2 kernel written in Bass

```python
import jax, jax.numpy as jnp
from jax import lax
import numpy as np

D_MODEL = 1024
BATCH = 16
SEQ = 2048
DEPTH = 2

HEAD_DIM = 64
N_ATTN_HEADS = D_MODEL // (2 * HEAD_DIM)
N_GMLP_GROUPS = D_MODEL // (2 * HEAD_DIM)
ATTN_WIDTH = N_ATTN_HEADS * HEAD_DIM
GMLP_WIDTH = N_GMLP_GROUPS * HEAD_DIM
MIX_WIDTH = ATTN_WIDTH + GMLP_WIDTH
IN_PROJ_WIDTH = 3 * ATTN_WIDTH + 2 * GMLP_WIDTH
MOBA_BLOCK = 256
MOBA_TOP_K = 3
QUERY_CHUNK = 16
GMLP_CHUNK = 128
D_FF = ((8 * D_MODEL // 3 + 255) // 256) * 256
PLE_DIM = 256
RMS_EPS = 1e-6
LN_EPS = 1e-5
NEG_INF = -1e30

kernel_name = "hybrid_moba_gmlp_macaron_trunk"


def rms_norm(x, g):
    xf = x.astype(jnp.float32)
    y = xf * lax.rsqrt(jnp.mean(xf * xf, axis=-1, keepdims=True) + RMS_EPS)
    return (y * g.astype(jnp.float32)).astype(x.dtype)


def swiglu_ffn(x, w_gate, w_up, w_down):
    return (jax.nn.silu(x @ w_gate) * (x @ w_up)) @ w_down


def alibi_slopes(n_heads):
    start = 2.0 ** (-8.0 / n_heads)
    return jnp.asarray(np.array([start ** (i + 1) for i in range(n_heads)], dtype=np.float32))


def moba_attention(q, k, v):
    B, H, S, dh = q.shape
    nb = -(-S // MOBA_BLOCK)
    pad = nb * MOBA_BLOCK - S
    padding = ((0, 0), (0, 0), (0, pad), (0, 0))
    k_blocks = jnp.pad(k, padding).reshape(B, H, nb, MOBA_BLOCK, dh)
    v_blocks = jnp.pad(v, padding).reshape(B, H, nb, MOBA_BLOCK, dh)
    slopes = alibi_slopes(H)
    scale = dh ** -0.5

    k_mean = jnp.mean(k_blocks.astype(jnp.float32), axis=3)
    gate = jnp.einsum("bhsd,bhnd->bhsn", q.astype(jnp.float32), k_mean)
    pos = jnp.arange(S)
    fully_past = jnp.arange(nb)[None, :] < (pos // MOBA_BLOCK)[:, None]
    gate = jnp.where(fully_past[None, None], gate, NEG_INF)
    n_sel = min(MOBA_TOP_K, nb)
    top_vals, top_idx = lax.top_k(gate, n_sel)
    sel_valid = top_vals > 0.5 * NEG_INF

    nqc = S // QUERY_CHUNK

    def to_chunks(a):
        a = a.reshape((B, H, nqc, QUERY_CHUNK) + a.shape[3:])
        return jnp.moveaxis(a, 2, 0)

    b_ix = jnp.arange(B)[:, None, None, None]
    h_ix = jnp.arange(H)[None, :, None, None]
    blk_off = jnp.arange(MOBA_BLOCK)
    n_sel_keys = n_sel * MOBA_BLOCK

    def one_chunk(args):
        q_c, idx_c, valid_c, c = args
        t = c * QUERY_CHUNK + jnp.arange(QUERY_CHUNK)
        own = (c * QUERY_CHUNK) // MOBA_BLOCK
        k_own = lax.dynamic_index_in_dim(k_blocks, own, axis=2, keepdims=False)
        v_own = lax.dynamic_index_in_dim(v_blocks, own, axis=2, keepdims=False)
        s_own = own * MOBA_BLOCK + blk_off
        l_own = jnp.einsum("bhqd,bhkd->bhqk", q_c, k_own).astype(jnp.float32) * scale
        d_own = (t[:, None] - s_own[None, :]).astype(jnp.float32)
        l_own = l_own - slopes[None, :, None, None] * d_own[None, None]
        l_own = jnp.where((s_own[None, :] <= t[:, None])[None, None], l_own, NEG_INF)
        k_sel = k_blocks[b_ix, h_ix, idx_c]
        v_sel = v_blocks[b_ix, h_ix, idx_c]
        l_sel = jnp.einsum("bhqd,bhqnkd->bhqnk", q_c, k_sel).astype(jnp.float32) * scale
        s_sel = idx_c[..., None] * MOBA_BLOCK + blk_off
        d_sel = (t[None, None, :, None, None] - s_sel).astype(jnp.float32)
        l_sel = l_sel - slopes[None, :, None, None, None] * d_sel
        l_sel = jnp.where(valid_c[..., None], l_sel, NEG_INF)
        logits = jnp.concatenate([l_sel.reshape(B, H, QUERY_CHUNK, n_sel_keys), l_own], axis=-1)
        probs = jax.nn.softmax(logits, axis=-1).astype(v.dtype)
        p_sel = probs[..., :n_sel_keys].reshape(B, H, QUERY_CHUNK, n_sel, MOBA_BLOCK)
        p_own = probs[..., n_sel_keys:]
        return (jnp.einsum("bhqnk,bhqnkd->bhqd", p_sel, v_sel)
                + jnp.einsum("bhqk,bhkd->bhqd", p_own, v_own))

    out = lax.map(one_chunk, (to_chunks(q), to_chunks(top_idx), to_chunks(sel_valid),
                              jnp.arange(nqc)))
    return jnp.moveaxis(out, 0, 2).reshape(B, H, S, dh)


def chunked_spatial_gating(u, v, ln_g, ln_b, w_s, b_s):
    B, S, G, dg = v.shape
    vf = v.astype(jnp.float32)
    mu = jnp.mean(vf, axis=-1, keepdims=True)
    var = jnp.mean(jnp.square(vf - mu), axis=-1, keepdims=True)
    vn = ((vf - mu) * lax.rsqrt(var + LN_EPS) * ln_g.reshape(G, dg).astype(jnp.float32)
          + ln_b.reshape(G, dg).astype(jnp.float32)).astype(v.dtype)
    nc = S // GMLP_CHUNK
    causal = jnp.tril(jnp.ones((GMLP_CHUNK, GMLP_CHUNK), dtype=w_s.dtype))
    mixed = jnp.einsum("gts,bcsgd->bctgd", w_s * causal,
                       vn.reshape(B, nc, GMLP_CHUNK, G, dg))
    mixed = mixed + b_s.T[None, None, :, :, None]
    return u * mixed.reshape(B, S, G, dg)


def token_mixer(hn, w_in, gmlp_ln_g, gmlp_ln_b, gmlp_w_s, gmlp_b_s,
                attn_out_norm, gmlp_out_norm, w_out):
    B, S, _ = hn.shape
    z = hn @ w_in
    q, k, v, gu, gv = jnp.split(
        z, [ATTN_WIDTH, 2 * ATTN_WIDTH, 3 * ATTN_WIDTH, 3 * ATTN_WIDTH + GMLP_WIDTH], axis=-1)

    def heads(a):
        return a.reshape(B, S, N_ATTN_HEADS, HEAD_DIM).transpose(0, 2, 1, 3)

    attn = moba_attention(heads(q), heads(k), heads(v))
    attn = attn.transpose(0, 2, 1, 3).reshape(B, S, ATTN_WIDTH)

    gu = jax.nn.gelu(gu, approximate=False).reshape(B, S, N_GMLP_GROUPS, HEAD_DIM)
    gv = jax.nn.gelu(gv, approximate=False).reshape(B, S, N_GMLP_GROUPS, HEAD_DIM)
    g = chunked_spatial_gating(gu, gv, gmlp_ln_g, gmlp_ln_b, gmlp_w_s, gmlp_b_s)
    g = g.reshape(B, S, GMLP_WIDTH)

    merged = jnp.concatenate([rms_norm(attn, attn_out_norm), rms_norm(g, gmlp_out_norm)], axis=-1)
    return merged @ w_out


def setup_inputs(seed: int = 0) -> dict:
    key = jax.random.key(seed)
    ks = iter(jax.random.split(key, 32))

    def nrm(shape, scale):
        return jax.random.normal(next(ks), shape, jnp.float32) * scale

    def gain(n):
        return 1.0 + nrm((DEPTH, n), 0.05)

    return {
        "x": nrm((BATCH, SEQ, D_MODEL), 1.0),
        "p": nrm((DEPTH, BATCH, SEQ, PLE_DIM), 1.0),
        "ffn1_pre_norm": gain(D_MODEL),
        "ffn1_w_gate": nrm((DEPTH, D_MODEL, D_FF), D_MODEL ** -0.5),
        "ffn1_w_up": nrm((DEPTH, D_MODEL, D_FF), D_MODEL ** -0.5),
        "ffn1_w_down": nrm((DEPTH, D_FF, D_MODEL), D_FF ** -0.5),
        "ffn1_post_norm": gain(D_MODEL),
        "mix_pre_norm": gain(D_MODEL),
        "w_in": nrm((DEPTH, D_MODEL, IN_PROJ_WIDTH), D_MODEL ** -0.5),
        "gmlp_ln_g": gain(GMLP_WIDTH),
        "gmlp_ln_b": nrm((DEPTH, GMLP_WIDTH), 0.02),
        "gmlp_w_s": nrm((DEPTH, N_GMLP_GROUPS, GMLP_CHUNK, GMLP_CHUNK), GMLP_CHUNK ** -0.5),
        "gmlp_b_s": 1.0 + nrm((DEPTH, N_GMLP_GROUPS, GMLP_CHUNK), 0.1),
        "attn_out_norm": gain(ATTN_WIDTH),
        "gmlp_out_norm": gain(GMLP_WIDTH),
        "w_out": nrm((DEPTH, MIX_WIDTH, D_MODEL), MIX_WIDTH ** -0.5),
        "mix_post_norm": gain(D_MODEL),
        "ffn2_pre_norm": gain(D_MODEL),
        "ffn2_w_gate": nrm((DEPTH, D_MODEL, D_FF), D_MODEL ** -0.5),
        "ffn2_w_up": nrm((DEPTH, D_MODEL, D_FF), D_MODEL ** -0.5),
        "ffn2_w_down": nrm((DEPTH, D_FF, D_MODEL), D_FF ** -0.5),
        "ffn2_post_norm": gain(D_MODEL),
        "ple_pre_norm": gain(D_MODEL),
        "ple_w_gate": nrm((DEPTH, D_MODEL, D_MODEL), D_MODEL ** -0.5),
        "ple_w_proj": nrm((DEPTH, PLE_DIM, D_MODEL), PLE_DIM ** -0.5),
        "ple_post_norm": gain(D_MODEL),
    }


def reference(x, p,
              ffn1_pre_norm, ffn1_w_gate, ffn1_w_up, ffn1_w_down, ffn1_post_norm,
              mix_pre_norm, w_in, gmlp_ln_g, gmlp_ln_b, gmlp_w_s, gmlp_b_s,
              attn_out_norm, gmlp_out_norm, w_out, mix_post_norm,
              ffn2_pre_norm, ffn2_w_gate, ffn2_w_up, ffn2_w_down, ffn2_post_norm,
              ple_pre_norm, ple_w_gate, ple_w_proj, ple_post_norm):
    h = x
    for i in range(DEPTH):
        f1 = swiglu_ffn(rms_norm(h, ffn1_pre_norm[i]), ffn1_w_gate[i], ffn1_w_up[i], ffn1_w_down[i])
        h = h + 0.5 * rms_norm(f1, ffn1_post_norm[i])
        m = token_mixer(rms_norm(h, mix_pre_norm[i]), w_in[i], gmlp_ln_g[i], gmlp_ln_b[i],
                        gmlp_w_s[i], gmlp_b_s[i], attn_out_norm[i], gmlp_out_norm[i], w_out[i])
        h = h + rms_norm(m, mix_post_norm[i])
        f2 = swiglu_ffn(rms_norm(h, ffn2_pre_norm[i]), ffn2_w_gate[i], ffn2_w_up[i], ffn2_w_down[i])
        h = h + 0.5 * rms_norm(f2, ffn2_post_norm[i])
        gate = jax.nn.sigmoid(rms_norm(h, ple_pre_norm[i]) @ ple_w_gate[i])
        e = gate * (p[i] @ ple_w_proj[i])
        h = h + rms_norm(e, ple_post_norm[i])
    return h
```

```python
import math
from contextlib import ExitStack

import numpy as np
import concourse.bass as bass
import concourse.mybir as mybir
from concourse.bass_utils import run_bass_kernel_spmd

F32 = mybir.dt.float32
BF16 = mybir.dt.bfloat16
AF = mybir.ActivationFunctionType
ALU = mybir.AluOpType
AX = mybir.AxisListType

N_CORES = 8
L = 2
NS = 2
S = 2048
D = 1024
KC = 8
TC = 512
NCH = S // TC
DFF = 2816
G = 2
NG = DFF // (128 * G)
PL = 72
RMS_EPS = 1e-6
LN_EPS = 1e-5
BIG = 30000.0

ENGS = ["pe", "act", "dve", "pool", "sp"]
import os
SAME_ENG_SYNC = os.environ.get("MK_SES", "1") == "1"
RING = 4
DMA_RING = 8
RES_ENG = os.environ.get("MK_RES", "dve")
LNEXP = os.environ.get("MK_LNEXP", "1") == "1"
ACC_SPLIT = os.environ.get("MK_ACCSPLIT", "1") == "1"


class Buf:
    __slots__ = ("name", "writer", "rd_eng", "rd_dma")

    def __init__(self, name):
        self.name = name
        self.writer = None
        self.rd_eng = {}
        self.rd_dma = []


class Op:
    __slots__ = ("eng", "fn", "dma", "deps", "need_inc", "inc_no", "dma_no", "waits", "know")

    def __init__(self, eng, fn, dma):
        self.eng = eng
        self.fn = fn
        self.dma = dma
        self.deps = set()
        self.need_inc = False
        self.inc_no = 0
        self.dma_no = -1
        self.waits = []
        self.know = None


class T:
    __slots__ = ("ap", "bufs")

    def __init__(self, ap, bufs):
        self.ap = ap
        self.bufs = list(bufs)

    def __getitem__(self, k):
        return T(self.ap[k], self.bufs)

    def v(self, fn):
        return T(fn(self.ap), self.bufs)


class Region:
    def __init__(self, nc, es, name, nbytes, page=1024):
        self.t = es.enter_context(nc.sbuf_tensor(name, [128, nbytes // 2], BF16))
        self.page = page
        self.bufs = [Buf(f"{name}{i}") for i in range((nbytes + page - 1) // page)]

    def tile(self, off, nbytes, dtype=BF16):
        ap = self.t[:, off // 2:(off + nbytes) // 2]
        if dtype is F32:
            ap = ap.bitcast(F32)
        b0 = off // self.page
        b1 = (off + nbytes + self.page - 1) // self.page
        return T(ap, self.bufs[b0:b1])


class Prog:
    def __init__(self, nc):
        self.nc = nc
        self.ops = []
        self.eng_ops = {e: [] for e in ENGS}
        self.ndma = {"sp": 0, "pool": 0}

    def add(self, eng, fn, reads=(), writes=(), dma=False):
        op = Op(eng, fn, dma)
        deps = op.deps
        for t in reads:
            for b in t.bufs:
                if b.writer is not None:
                    deps.add(b.writer)
        for t in writes:
            for b in t.bufs:
                if b.writer is not None:
                    deps.add(b.writer)
                deps.update(b.rd_eng.values())
                deps.update(b.rd_dma)
        for t in reads:
            for b in t.bufs:
                if dma:
                    b.rd_dma.append(op)
                else:
                    b.rd_eng[eng] = op
        for t in writes:
            for b in t.bufs:
                b.writer = op
                b.rd_eng = {}
                b.rd_dma = []
        deps.discard(op)
        if dma:
            op.dma_no = self.ndma[eng]
            self.ndma[eng] += 1
        self.ops.append(op)
        self.eng_ops[eng].append(op)
        return op

    def mm(self, out, lhsT, rhs, start=True, stop=True):
        rd = [lhsT, rhs] + ([] if start else [out])
        return self.add("pe", lambda e: e.matmul(out.ap, lhsT.ap, rhs.ap, start=start, stop=stop), rd, [out])

    def act(self, out, in_, func, bias=0.0, scale=1.0, extra_reads=()):
        b = bias.ap if isinstance(bias, T) else bias
        rd = [in_] + ([bias] if isinstance(bias, T) else []) + list(extra_reads)
        return self.add("act", lambda e: e.activation(out=out.ap, in_=in_.ap, func=func, bias=b, scale=scale), rd, [out])

    def tt(self, out, in0, in1, op, eng="dve"):
        return self.add(eng, lambda e: e.tensor_tensor(out=out.ap, in0=in0.ap, in1=in1.ap, op=op), [in0, in1], [out])

    def stt(self, out, in0, scalar, in1, op0, op1, eng="dve"):
        s = scalar.ap if isinstance(scalar, T) else scalar
        rd = [in0, in1] + ([scalar] if isinstance(scalar, T) else [])
        return self.add(eng, lambda e: e.scalar_tensor_tensor(out=out.ap, in0=in0.ap, scalar=s, in1=in1.ap, op0=op0, op1=op1), rd, [out])

    def ts(self, out, in0, s1, s2, op0, op1=None, eng="dve"):
        if op1 is None:
            return self.add(eng, lambda e: e.tensor_scalar(out=out.ap, in0=in0.ap, scalar1=s1, scalar2=None, op0=op0), [in0], [out])
        return self.add(eng, lambda e: e.tensor_scalar(out=out.ap, in0=in0.ap, scalar1=s1, scalar2=s2, op0=op0, op1=op1), [in0], [out])

    def copy(self, out, in_, eng="dve"):
        return self.add(eng, lambda e: e.tensor_copy(out=out.ap, in_=in_.ap), [in_], [out])

    def recip(self, out, in_):
        return self.add("dve", lambda e: e.reciprocal(out=out.ap, in_=in_.ap), [in_], [out])

    def reduce(self, out, in_, op=ALU.add):
        return self.add("dve", lambda e: e.tensor_reduce(out=out.ap, in_=in_.ap, axis=AX.X, op=op), [in_], [out])

    def memset(self, out, val, eng="dve"):
        return self.add(eng, lambda e: e.memset(out.ap, val), [], [out])

    def dma(self, out, in_, q="sp", reads=(), writes=()):
        return self.add(q, lambda e: e.dma_start(out=out, in_=in_), reads, writes, dma=True)

    def wait_all(self, eng, ops):
        op = Op(eng, None, False)
        op.deps = set(ops)
        self.ops.append(op)
        self.eng_ops[eng].append(op)
        return op

    def finalize(self, es):
        nc = self.nc
        for op in self.ops:
            for d in op.deps:
                if d.dma:
                    continue
                if d.eng == op.eng and not op.dma and (op.eng == "pe" or not SAME_ENG_SYNC):
                    continue
                d.need_inc = True
        cnt = {e: 0 for e in ENGS}
        for op in self.ops:
            if op.need_inc:
                cnt[op.eng] += 1
                op.inc_no = cnt[op.eng]
        self.sems = {e: [es.enter_context(nc.semaphore(f"s_{e}{i}")) for i in range(RING)] for e in ENGS}
        self.dsems = {q: [es.enter_context(nc.semaphore(f"d_{q}{i}")) for i in range(DMA_RING)] for q in ("sp", "pool")}
        know = {e: {} for e in ENGS}
        know_dma = {e: set() for e in ENGS}
        nwaits = 0
        for op in self.ops:
            e = op.eng
            K = know[e]
            if op.dma and op.dma_no >= DMA_RING:
                n = op.dma_no - DMA_RING
                key = (e, n)
                if key not in know_dma[e]:
                    op.waits.append((self.dsems[e][n % DMA_RING], 16 * (n // DMA_RING + 1)))
                    know_dma[e].add(key)
            for d in sorted(op.deps, key=lambda o: (o.eng, o.inc_no, o.dma_no)):
                if d.dma:
                    key = (d.eng, d.dma_no)
                    if key in know_dma[e]:
                        continue
                    op.waits.append((self.dsems[d.eng][d.dma_no % DMA_RING], 16 * (d.dma_no // DMA_RING + 1)))
                    know_dma[e].add(key)
                    continue
                if d.eng == e and not op.dma and (e == "pe" or not SAME_ENG_SYNC):
                    continue
                if K.get(d.eng, 0) >= d.inc_no:
                    continue
                n = d.inc_no - 1
                op.waits.append((self.sems[d.eng][n % RING], n // RING + 1))
                for k2, v2 in d.know.items():
                    if K.get(k2, 0) < v2:
                        K[k2] = v2
            nwaits += len(op.waits)
            if op.need_inc:
                kk = dict(K)
                kk[e] = max(kk.get(e, 0), op.inc_no)
                op.know = kk
        self.nwaits = nwaits

    def emit(self, es):
        nc = self.nc
        block = es.enter_context(nc.Block())
        prog = self

        def run(name, e):
            sems = prog.sems[name]
            with nc.allow_low_precision("bf16 matmul operands, fp32 accumulation"):
                for op in prog.eng_ops[name]:
                    for (sem, val) in op.waits:
                        e.wait_ge(sem, val)
                    if op.fn is None:
                        continue
                    ins = op.fn(e)
                    if op.dma:
                        ins.then_inc(prog.dsems[name][op.dma_no % DMA_RING], 16)
                    elif op.need_inc:
                        ins.then_inc(sems[(op.inc_no - 1) % RING], 1)

        @block.tensor
        def _(e):
            run("pe", e)

        @block.scalar
        def _(e):
            run("act", e)

        @block.vector
        def _(e):
            run("dve", e)

        @block.gpsimd
        def _(e):
            run("pool", e)

        @block.sync
        def _(e):
            run("sp", e)


def pcol(l, name):
    base = l * PL
    return base + {"ffn1_pre": 0, "ffn1_post": 8, "mix_pre": 16, "mix_post": 24, "ffn2_pre": 32, "ffn2_post": 40,
                   "ple_pre": 48, "ple_post": 56, "attn_out": 64, "gmlp_out": 68}[name]


class Builder:
    def __init__(self, n_seq=NS, n_layers=L, stages=("ffn1", "mix", "ffn2", "ple")):
        self.n_seq = n_seq
        self.n_layers = n_layers
        self.stages = stages
        self.nc = bass.Bass("TRN2", target_bir_lowering=False)
        self.es = ExitStack()
        self.P = Prog(self.nc)
        self.declare()
        self.alloc()
        self.program()
        self.P.finalize(self.es)
        self.P.emit(self.es)
        self.es.close()

    def declare(self):
        nc = self.nc
        ns = self.n_seq

        def inp(name, shape):
            return nc.dram_tensor(name, list(shape), F32, kind="ExternalInput").ap()

        self.xT = inp("xT", [ns, 128, 8, S])
        self.pT = inp("pT", [L, ns, 128, 2, S])
        self.wg = inp("wg", [L * 2, NG, 128, 8, G * 128])
        self.wu = inp("wu", [L * 2, NG, 128, 8, G * 128])
        self.wd = inp("wd", [L * 2, NG, 128, G, D])
        self.wvg = inp("wvg", [L, 2, 128, 8, 512])
        self.wgu = inp("wgu", [L, 128, 8, 512])
        self.wqk = inp("wqk", [L, 4, 128, 8, 256])
        self.wo = inp("wo", [L, 128, 8, 1024])
        self.wpg = inp("wpg", [L, 8, 128, 8, 128])
        self.wpp = inp("wpp", [L, 128, 2, 1024])
        self.wsT = inp("wsT", [L, 128, 8 * 128])
        self.bsd = inp("bs", [L, 1, 1024])
        self.lng = inp("lng", [L, 128, 512])
        self.lnb = inp("lnb", [L, 128, 512])
        self.ppd = inp("pp", [128, L * PL])
        self.cbd = inp("cb", [128, 512])
        self.cfd = inp("cf", [128, 256])
        self.qaugd = inp("qaug", [8, 2, S])
        self.kaugd = inp("kaug", [10, S])
        self.outT = nc.dram_tensor("outT", [ns, 128, 8, S], F32, kind="ExternalOutput").ap()

    def alloc(self):
        nc, es = self.nc, self.es
        self.RH = Region(nc, es, "hT", 65536, page=2048)
        self.RB = Region(nc, es, "big", 65536)
        self.RC = Region(nc, es, "rc", 32768)
        self.RW = Region(nc, es, "rw", 24576)
        self.RS = Region(nc, es, "rs", 16384)
        self.RK = Region(nc, es, "rk", 7168, page=256)
        self.banks = []
        for i in range(8):
            t = es.enter_context(nc.psum_tensor(f"ps{i}", [128, 512], F32))
            self.banks.append(T(t[:, :], [Buf(f"ps{i}")]))
        RK = self.RK
        self.ones = RK.tile(0, 256)
        self.ident = RK.tile(256, 256)
        self.trineg = RK.tile(512, 256)
        self.causT = RK.tile(768, 256)
        self.cf = RK.tile(1024, 1024, F32)
        self.pp = RK.tile(2048, 1024, F32)
        self.pph = RK.tile(3072, 1024, F32)

    def h(self, m, c):
        return self.RH.tile((m * S + c * TC) * 4, TC * 4, F32)

    def acc(self, m, c):
        return self.RB.tile((c * 8 + m) * 2048, 2048, F32)

    def hnF(self, kc, c):
        return self.RC.tile((c * 8 + kc) * 1024, 1024)

    HN_SLOT = (0, 1, 3, 5)
    MG_SLOT = (2, 4, 6, 7)

    def hnM(self, kc, c):
        return self.RB.tile(self.HN_SLOT[c] * 8192 + kc * 1024, 1024)

    def merged(self, kc, c):
        return self.RB.tile(self.MG_SLOT[c] * 8192 + kc * 1024, 1024)

    def xsM(self, m, c):
        if c % 2 == 0:
            return self.RB.tile(m * 2048, 2048, F32)
        slot = 3 if m < 4 else 5
        return self.RB.tile(slot * 8192 + (m % 4) * 2048, 2048, F32)

    def hview(self, c):
        return T(self.RH.t[:, :].bitcast(F32).rearrange("p (m t) -> p m t", m=8)[:, :, c * TC:(c + 1) * TC],
                 [b for m in range(8) for b in self.h(m, c).bufs])

    def program(self):
        P = self.P
        P.dma(self.RK.t[:, 0:512], self.cbd, q="pool", writes=[self.ones, self.ident, self.trineg, self.causT])
        P.dma(self.cf.ap, self.cfd, q="sp", writes=[self.cf])
        P.dma(self.pp.ap[:, 0:L * PL], self.ppd, q="sp", writes=[self.pp])
        P.ts(self.pph[:, 0:L * PL], self.pp[:, 0:L * PL], 0.5, None, ALU.mult)
        self.cnt = 0
        self.dcnt = 0
        self.rcnt = 0
        self.tickno = 0
        self.pending = []
        self.out_dmas = []
        self.ffn_base = 0
        self.mix_pre = None
        self.ple_pre = None
        self.early_hook = None
        for s in range(self.n_seq):
            blocks = []
            for l in range(self.n_layers):
                for st in ("ffn1", "mix", "ffn2", "ple"):
                    if st in self.stages:
                        blocks.append((st, l))
            self.cur_s = s
            self.set_next(blocks, -1)
            for c in range(NCH):
                self.final(c, None, None, False, start=True)
                self.tick()
            for bi, (st, l) in enumerate(blocks):
                self.set_next(blocks, bi)
                self.early_hook = None
                if st in ("ffn1", "ffn2") and bi + 1 < len(blocks) and blocks[bi + 1][0] in ("mix", "ple"):
                    self.early_hook = (lambda fslot, nb=blocks[bi + 1]: self.early_load(nb[0], nb[1], fslot))
                if st == "ffn1":
                    self.ffn(l, 0)
                elif st == "mix":
                    self.mixer(l, s)
                elif st == "ffn2":
                    self.ffn(l, 1)
                else:
                    self.ple(l, s)
            self.flush()
        P.wait_all("sp", self.out_dmas)

    def set_next(self, blocks, bi):
        if bi + 1 < len(blocks):
            st, l = blocks[bi + 1]
            name = {"ffn1": "ffn1_pre", "mix": "mix_pre", "ffn2": "ffn2_pre", "ple": "ple_pre"}[st]
            self.next_pre = (pcol(l, name), self.hnM if st == "mix" else self.hnF)
        else:
            self.next_pre = None
        self.sq_ring_mode = False
        self.next_is_mix = (bi + 1 < len(blocks) and blocks[bi + 1][0] == "mix")

    def dbank(self):
        b = self.banks[4 + self.dcnt % 4]
        self.dcnt += 1
        return b

    def sq_t(self, i):
        return self.RS.tile(14336 + (i % 2) * 1024, 1024)

    def rstd_t(self):
        self.rcnt += 1
        return self.RS.tile(8192 + (self.rcnt % 2) * 2048, 2048, F32)

    def rms_stats(self, tiles, nfeat, eps):
        pr = self.P
        bank = self.dbank()
        n = len(tiles)
        for i, t in enumerate(tiles):
            sq = self.sq_t(self.cnt)
            self.cnt += 1
            pr.act(sq, t, AF.Square)
            pr.mm(bank, self.ones, sq, start=(i == 0), stop=(i == n - 1))
        r = self.rstd_t()
        if LNEXP:
            pr.act(r, bank, AF.Ln, bias=float(eps), scale=1.0 / nfeat)
            pr.act(r, r, AF.Exp, scale=-0.5)
        else:
            pr.act(r, bank, AF.Sqrt, bias=float(eps), scale=1.0 / nfeat)
            pr.recip(r, r)
        return r

    def tick(self):
        self.tickno += 1
        keep = []
        for item in self.pending:
            if item[0] <= self.tickno:
                item[2]()
            else:
                keep.append(item)
        self.pending = keep

    def need(self, c):
        keep = []
        for item in self.pending:
            if item[1] <= c:
                item[2]()
            else:
                keep.append(item)
        self.pending = keep

    def flush(self):
        for item in self.pending:
            item[2]()
        self.pending = []

    def stats_from(self, sqs):
        pr = self.P
        bank = self.dbank()
        for m in range(8):
            pr.mm(bank, self.ones, sqs[m], start=(m == 0), stop=(m == 7))
        r = self.rstd_t()
        if LNEXP:
            pr.act(r, bank, AF.Ln, bias=float(RMS_EPS), scale=1.0 / D)
            pr.act(r, r, AF.Exp, scale=-0.5)
        else:
            pr.act(r, bank, AF.Sqrt, bias=float(RMS_EPS), scale=1.0 / D)
            pr.recip(r, r)
        return r

    def final(self, c, src, gcol_post, half, start=False):
        pr = self.P
        nxt = self.next_pre
        ring = self.next_is_mix and c == 3
        sqb = [self.hnF(m, c) for m in range(8)]
        s = self.cur_s

        def squares(tiles):
            for m in range(8):
                pr.act(sqb[m], tiles[m], AF.Square)

        def pre_part():
            gcol, dst = nxt
            if ring:
                r = self.rms_stats([self.h(m, c) for m in range(8)], D, RMS_EPS)
            else:
                r = self.stats_from(sqb)
            for m in range(8):
                pr.stt(dst(m, c), self.h(m, c), self.pp[:, gcol + m:gcol + m + 1], r, ALU.mult, ALU.mult)

        def post_part():
            srcs = [src(m) for m in range(8)]
            if ring:
                r = self.rms_stats(srcs, D, RMS_EPS)
            else:
                r = self.stats_from(sqb)
            gp = self.pph if half else self.pp
            for m in range(8):
                pr.stt(srcs[m], srcs[m], gp[:, gcol_post + m:gcol_post + m + 1], r, ALU.mult, ALU.mult)
            for m in range(8):
                pr.tt(self.h(m, c), self.h(m, c), srcs[m], ALU.add, eng=RES_ENG)
            if nxt is not None:
                if not ring:
                    squares([self.h(m, c) for m in range(8)])
            else:
                hv = self.hview(c)
                self.out_dmas.append(pr.dma(self.outT[s, :, :, c * TC:(c + 1) * TC], hv.ap, q="sp", reads=[hv]))

        if start:
            hv = self.hview(c)
            pr.dma(hv.ap, self.xT[s, :, :, c * TC:(c + 1) * TC], q="sp", writes=[hv])
            if nxt is not None:
                squares([self.h(m, c) for m in range(8)])
                self.pending.append((self.tickno + 2, c, pre_part))
            return
        if not ring:
            squares([src(m) for m in range(8)])
        self.pending.append((self.tickno + 2, c, post_part))
        if nxt is not None:
            self.pending.append((self.tickno + 4, c, pre_part))

    def early_load(self, st, l, fslot):
        pr = self.P
        RW = self.RW
        r3 = lambda a, k: a.rearrange("p (k n) -> p k n", k=k)
        if st == "mix":
            wv = RW.tile(fslot * 8192, 8192)
            pr.dma(r3(wv.ap, 8), self.wvg[l, 0], q="pool", writes=[wv])
            self.mix_pre = fslot
        else:
            for m in range(4 * fslot, 4 * fslot + 4):
                w = RW.tile(m * 2048, 2048)
                pr.dma(r3(w.ap, 8), self.wpg[l, m], q="pool", writes=[w])
            wpp = RW.tile(16384 + fslot * 4096, 4096)
            pr.dma(r3(wpp.ap, 2), self.wpp[l], q="pool", writes=[wpp])
            self.ple_pre = fslot

    def ffn(self, l, which):
        pr = self.P
        f = l * 2 + which
        post = pcol(l, "ffn1_post" if which == 0 else "ffn2_post")
        RW, RS = self.RW, self.RS
        base = self.ffn_base

        def wgu_t(gi):
            return RW.tile(((gi + base) % 2) * 8192, 8192)

        def wd_t(gi):
            return RW.tile(16384 + ((gi + base) % 2) * 4096, 4096)

        def load(gi):
            w = wgu_t(gi)
            v = w.ap.rearrange("p (a k n) -> p a k n", a=2, k=8)
            pr.dma(v[:, 0], self.wg[f, gi], q="pool", writes=[w])
            pr.dma(v[:, 1], self.wu[f, gi], q="pool", writes=[w])
            wdt = wd_t(gi)
            pr.dma(wdt.ap.rearrange("p (j n) -> p j n", j=G), self.wd[f, gi], q="pool", writes=[wdt])

        load(0)
        steps = [(gi, c) for gi in range(NG) for c in range(NCH)]
        hid = 0

        def sg_t(i):
            return RS.tile((i % 2) * 2048, 2048, F32)

        def act_t(i, jj):
            return RS.tile(4096 + (i % 2) * 2048 + jj * 1024, 1024)

        def GU(i):
            nonlocal hid
            gi, c = steps[i]
            w = wgu_t(gi).v(lambda a: a.rearrange("p (a k n) -> p a k n", a=2, k=8))
            for jj in range(G):
                bg = self.banks[(hid % 2) * 2]
                bu = self.banks[(hid % 2) * 2 + 1]
                for kc in range(KC):
                    pr.mm(bg, w[:, 0, kc, jj * 128:(jj + 1) * 128], self.hnF(kc, c), start=(kc == 0), stop=(kc == KC - 1))
                for kc in range(KC):
                    pr.mm(bu, w[:, 1, kc, jj * 128:(jj + 1) * 128], self.hnF(kc, c), start=(kc == 0), stop=(kc == KC - 1))
                sg = sg_t(hid)
                pr.act(sg, bg, AF.Silu)
                pr.tt(act_t(i, jj), sg, bu, ALU.mult)
                hid += 1

        def Dn(i):
            gi, c = steps[i]
            if gi == 0 and c == 0:
                self.need(NCH - 1)
            wdt = wd_t(gi).v(lambda a: a.rearrange("p (j n) -> p j n", j=G))
            for m in range(8):
                bank = self.dbank()
                for jj in range(G):
                    pr.mm(bank, wdt[:, jj, m * 128:(m + 1) * 128], act_t(i, jj), start=(jj == 0), stop=(jj == G - 1))
                if ACC_SPLIT and m % 2 == 1:
                    if gi == 0:
                        pr.act(self.acc(m, c), bank, AF.Identity)
                    else:
                        tmp = RS.tile(12288 + (self.cnt % 2) * 2048, 2048, F32)
                        self.cnt += 1
                        pr.act(tmp, bank, AF.Identity)
                        pr.tt(self.acc(m, c), tmp, self.acc(m, c), ALU.add, eng="pool")
                elif gi == 0:
                    pr.copy(self.acc(m, c), bank)
                else:
                    pr.tt(self.acc(m, c), bank, self.acc(m, c), ALU.add)
            if gi == NG - 1:
                self.final(c, lambda m, c=c: self.acc(m, c), post, True)

        for i in range(len(steps)):
            gi, c = steps[i]
            if gi == 0:
                self.need(c)
            GU(i)
            self.tick()
            if i >= 1:
                Dn(i - 1)
            if c == 0 and gi + 1 < NG:
                load(gi + 1)
            if c == 0 and gi == NG - 1 and self.early_hook is not None:
                self.early_hook((NG - 2 + base) % 2)
            self.tick()
        Dn(len(steps) - 1)
        self.ffn_base = (base + NG) % 2

    def mixer(self, l, s):
        pr = self.P
        RW, RS, RC, RB, RK = self.RW, self.RS, self.RC, self.RB, self.RK
        r3 = lambda a, k: a.rearrange("p (k n) -> p k n", k=k)
        fs = self.mix_pre
        self.mix_pre = None
        vslot = 0 if fs is None else fs
        wv = RW.tile(vslot * 8192, 8192)
        wgv = RW.tile((1 - vslot) * 8192, 8192)
        wgu = RW.tile(16384, 8192)
        if fs is None:
            pr.dma(r3(wv.ap, 8), self.wvg[l, 0], q="pool", writes=[wv])
        pr.dma(r3(wgv.ap, 8), self.wvg[l, 1], q="pool", writes=[wgv])
        pr.dma(r3(wgu.ap, 8), self.wgu[l], q="pool", writes=[wgu])
        wv3 = wv.v(lambda a: r3(a, 8))
        wgv3 = wgv.v(lambda a: r3(a, 8))
        wgu3 = wgu.v(lambda a: r3(a, 8))
        lng = RC.tile(24576, 2048, F32)
        lnb = RC.tile(26624, 2048, F32)
        wst = RC.tile(28672, 2048)
        bsb = RC.tile(30720, 2048)
        pr.dma(lng.ap, self.lng[l], q="sp", writes=[lng])
        pr.dma(lnb.ap, self.lnb[l], q="sp", writes=[lnb])
        pr.dma(wst.ap, self.wsT[l], q="pool", writes=[wst])
        pr.dma(bsb.ap[0:1, :], self.bsd[l], q="pool", writes=[bsb])
        wst3 = wst.v(lambda a: r3(a, 8))
        pr.tt(wst3, wst3, self.causT.v(lambda a: a.unsqueeze(1).to_broadcast([128, 8, 128])), ALU.mult)

        def Vt(t):
            return RC.tile(t * 1024, 1024)

        def vn_t(tt):
            return RS.tile(tt * 1024, 1024)

        def ge_t(i):
            return RS.tile(4096 + (i % 2) * 2048, 2048, F32)

        def small(i, k):
            return RK.tile(6144 + ((i % 4) * 8 + k) * 32, 32, F32)

        u_t = RK.tile(4096, 2048, F32)
        g3 = lambda a: a.rearrange("p (g d) -> p g d", g=8)
        bc3 = lambda a: a.unsqueeze(2).to_broadcast([128, 8, 64])

        def ge4(tt):
            return RS.tile(4096 + tt * 2048, 2048, F32)

        def ub(mc):
            return RS.tile(12288 + mc * 1024, 1024) if mc < 2 else RK.tile(4096 + (mc - 2) * 1024, 1024)

        s2all = RK.tile(6144 + 512, 128, F32)
        rsall = RK.tile(6144 + 640, 128, F32)
        for c in range(NCH):
            self.need(c)
            for tt in range(4):
                t = 4 * c + tt
                bv = self.banks[tt % 2]
                bg = self.banks[2 + tt % 2]
                for kc in range(KC):
                    pr.mm(bv, self.hnM(kc, c)[:, tt * 128:(tt + 1) * 128], wv3[:, kc, :], start=(kc == 0), stop=(kc == KC - 1))
                for kc in range(KC):
                    pr.mm(bg, self.hnM(kc, c)[:, tt * 128:(tt + 1) * 128], wgv3[:, kc, :], start=(kc == 0), stop=(kc == KC - 1))
                pr.act(Vt(t), bv, AF.Identity)
                ge = ge4(tt)
                pr.act(ge, bg, AF.Gelu)
                s1, mean = small(tt, 0), small(tt, 1)
                ge3 = ge.v(g3)
                pr.reduce(s1, ge3)
                pr.ts(mean, s1, 1.0 / 64, None, ALU.mult)
                pr.tt(ge3, ge3, mean.v(bc3), ALU.subtract)
            for mc in range(4):
                bu = self.banks[4 + mc]
                for kc in range(KC):
                    pr.mm(bu, wgu3[:, kc, mc * 128:(mc + 1) * 128], self.hnM(kc, c), start=(kc == 0), stop=(kc == KC - 1))
            for mc in range(4):
                pr.act(ub(mc), self.banks[4 + mc], AF.Gelu)
            for tt in range(4):
                sqb = self.sq_t(self.cnt)
                self.cnt += 1
                pr.act(sqb, ge4(tt), AF.Square)
                pr.reduce(s2all[:, tt * 8:(tt + 1) * 8], sqb.v(g3))
            pr.act(rsall, s2all, AF.Sqrt, bias=float(LN_EPS), scale=1.0 / 64)
            pr.recip(rsall, rsall)
            for tt in range(4):
                ge = ge4(tt)
                ge3 = ge.v(g3)
                pr.tt(ge3, ge3, rsall[:, tt * 8:(tt + 1) * 8].v(bc3), ALU.mult)
                pr.tt(ge, ge, lng, ALU.mult)
                pr.tt(vn_t(tt), ge, lnb, ALU.add)
            self.need(min(c + 1, NCH - 1))
            for mc in range(4):
                bsp = self.banks[mc]
                for e in range(2):
                    g = 2 * mc + e
                    for tt in range(4):
                        o = bsp[e * 64:(e + 1) * 64, tt * 128:(tt + 1) * 128]
                        pr.mm(o, vn_t(tt)[:, g * 64:(g + 1) * 64], wst[:, g * 128:(g + 1) * 128], start=True, stop=False)
                        pr.mm(o, self.ones[0:1, 0:64], bsb[0:1, g * 128:(g + 1) * 128], start=False, stop=True)
                pr.tt(self.merged(4 + mc, c), ub(mc), bsp, ALU.mult)
            r = self.rms_stats([self.merged(4 + mc, c) for mc in range(4)], 512, RMS_EPS)
            gcol = pcol(l, "gmlp_out")
            for mc in range(4):
                pr.stt(self.merged(4 + mc, c), self.merged(4 + mc, c), self.pp[:, gcol + mc:gcol + mc + 1], r, ALU.mult, ALU.mult)
            self.tick()
        self.flush()

        def wqk_t(p):
            return RW.tile(16384 + (p % 2) * 4096, 4096)

        def load_qk(p):
            w = wqk_t(p)
            pr.dma(r3(w.ap, 8), self.wqk[l, p], q="pool", writes=[w])

        wo = RW.tile(0, 16384)

        def qa_c(sl, c):
            return RS.tile(sl * 4096 + c * 1024, 1024)

        def ka_c(sl, c):
            return RS.tile(8192 + sl * 4096 + c * 1024, 1024)

        def qa_all(sl):
            return RS.tile(sl * 4096, 4096)

        def ka_all(sl):
            return RS.tile(8192 + sl * 4096, 4096)

        def Vaug(sl):
            return RC.tile(16384 + sl * 4096, 4096)

        def PT(i):
            return RC.tile(24576 + (i % 4) * 1024, 1024)

        rec = RC.tile(28672, 2048, F32)
        SM = 30720

        def gm_t(i):
            return RC.tile(SM + i * 32, 32, F32)

        def mx_t(i):
            return RC.tile(SM + 512 + i * 32, 32, F32)

        def pen_t(i):
            return RC.tile(SM + 1024 + i * 16, 16)

        def ksum_t(sl):
            return RC.tile(SM + 1280 + sl * 32, 32, F32)

        def kmean_t(sl):
            return RC.tile(SM + 1344 + sl * 16, 16)

        load_qk(0)
        load_qk(1)
        pr.dma(r3(wo.ap, 8), self.wo[l], q="pool", writes=[wo])
        wo3 = wo.v(lambda a: r3(a, 8))
        for sl in range(2):
            ka = ka_all(sl)
            pr.dma(ka.ap[64:74, :], self.kaugd, q="pool", writes=[ka])
            pr.memset(qa_all(sl)[64:72, 0:1024], 0.0)
            va = Vaug(sl).v(lambda a: a.rearrange("p (t n) -> p t n", t=16))
            if sl == 0:
                pr.memset(va[:, :, 64:128], 1.0)
            else:
                pr.memset(va[:, :, 0:64], 1.0)
        AB = self.cf
        scnt = 0
        ocnt = 0
        pcnt = 0
        Vall = T(RC.t[:, 0:8192].rearrange("p (t n) -> p t n", t=16), RC.bufs[0:16])
        for pair in range(4):
            w3 = wqk_t(pair).v(lambda a: r3(a, 8))
            for sl in range(2):
                pr.dma(qa_all(sl).ap[72:74, :], self.qaugd[2 * pair + sl], q="pool", writes=[qa_all(sl)])
            for c in range(NCH):
                bq = self.banks[5 if c % 2 == 0 else 3]
                bk = self.banks[6 if c % 2 == 0 else 4]
                for kc in range(KC):
                    pr.mm(bq, w3[:, kc, 0:128], self.hnM(kc, c), start=(kc == 0), stop=(kc == KC - 1))
                for kc in range(KC):
                    pr.mm(bk, w3[:, kc, 128:256], self.hnM(kc, c), start=(kc == 0), stop=(kc == KC - 1))
                pr.act(qa_c(0, c)[0:64, :], bq[0:64, :], AF.Identity)
                pr.copy(qa_c(1, c)[0:64, :], bq[64:128, :])
                pr.act(ka_c(0, c)[0:64, :], bk[0:64, :], AF.Identity)
                pr.copy(ka_c(1, c)[0:64, :], bk[64:128, :])
                pr.reduce(ksum_t(0)[0:64, 2 * c:2 * c + 2], bk[0:64, :].v(lambda a: a.rearrange("p (b n) -> p b n", b=2)))
                pr.reduce(ksum_t(1)[0:64, 2 * c:2 * c + 2], bk[64:128, :].v(lambda a: a.rearrange("p (b n) -> p b n", b=2)))
            if pair + 2 < 4:
                load_qk(pair + 2)
            gb = self.banks[7]
            for sl in range(2):
                pr.ts(kmean_t(sl)[0:64, :], ksum_t(sl)[0:64, :], 1.0 / 256, None, ALU.mult)
            for sl in range(2):
                for t in range(8, 16):
                    i = sl * 8 + t - 8
                    pr.mm(gb[:, i * 8:(i + 1) * 8], qa_c(sl, t // 4)[0:64, (t % 4) * 128:(t % 4 + 1) * 128], kmean_t(sl)[0:64, :])
            for sl in range(2):
                for t in range(8, 16):
                    i = sl * 8 + t - 8
                    b = t // 2
                    pr.tt(gm_t(i), gb[:, i * 8:(i + 1) * 8], self.cf[:, 128 + b * 8:128 + (b + 1) * 8], ALU.add)
            for i in range(16):
                gm, mx = gm_t(i), mx_t(i)
                pr.add("dve", (lambda gm=gm, mx=mx: (lambda e: e.max(out=mx.ap, in_=gm.ap)))(), [gm], [mx])
            for sl in range(2):
                for t in range(8, 16):
                    i = sl * 8 + t - 8
                    b = t // 2
                    pr.stt(pen_t(i), gm_t(i), mx_t(i)[:, 2:3], self.cf[:, 192 + b * 8:192 + (b + 1) * 8], ALU.is_lt, ALU.mult)

            def pen_rows():
                for sl in range(2):
                    for r in range(2):
                        tb = self.banks[5 + r]
                        for t4 in range(4):
                            t = 8 + 4 * r + t4
                            pr.mm(tb[64:72, t4 * 128:(t4 + 1) * 128], pen_t(sl * 8 + t - 8), self.ident)
                        pr.copy(qa_c(sl, 2 + r)[64:72, :], tb[64:72, :])

            for sl in range(2):
                h = 2 * pair + sl
                va = Vaug(sl).v(lambda a: a.rearrange("p (t n) -> p t n", t=16))
                dst = va[:, :, 0:64] if sl == 0 else va[:, :, 64:128]
                pr.copy(dst, Vall[:, :, h * 64:(h + 1) * 64])
            LA = 2
            queue = []

            def emit_pv(item):
                ob, va, pkt, ppt, poff, last, fin = item
                pr.mm(ob[:, poff:TC], va[:, pkt, :], ppt[:, poff:TC], start=(pkt == 0), stop=last)
                if fin is not None:
                    fin()

            for sl, c in [(0, 0), (0, 1), (1, 0), (1, 1), ("pen", None), (0, 2), (0, 3), (1, 2), (1, 3)]:
                if sl == "pen":
                    pen_rows()
                    continue
                if True:
                    h = 2 * pair + sl
                    va = Vaug(sl).v(lambda a: a.rearrange("p (t n) -> p t n", t=16))
                    nkt = 4 * c + 4
                    ob = self.banks[3 + ocnt % 2]
                    ocnt += 1
                    qc = qa_c(sl, c)
                    mt = self.merged(pair, c)

                    def fin(ob=ob, mt=mt, sl=sl):
                        if sl == 0:
                            pr.recip(rec[0:64, :], ob[64:128, :])
                            pr.tt(mt[0:64, :], ob[0:64, :], rec[0:64, :], ALU.mult)
                        else:
                            pr.recip(rec[64:128, :], ob[0:64, :])
                            pr.tt(mt[64:128, :], ob[64:128, :], rec[64:128, :], ALU.mult)

                    for kt in range(nkt):
                        sb = self.banks[scnt % 3]
                        scnt += 1
                        kk = ka_c(sl, kt // 4)[0:74, (kt % 4) * 128:(kt % 4 + 1) * 128]
                        diag = kt >= 4 * c
                        off = (kt - 4 * c) * 128 if diag else 0
                        if not diag:
                            pr.mm(sb, kk, qc[0:74, :])
                        else:
                            pr.mm(sb[:, off:off + 128], kk, qc[0:74, off:off + 128], start=True, stop=False)
                            pr.mm(sb[:, off:off + 128], self.ident, self.trineg, start=False, stop=True)
                            if off + 128 < TC:
                                pr.mm(sb[:, off + 128:TC], kk, qc[0:74, off + 128:TC])
                        pt = PT(pcnt)
                        pcnt += 1
                        d = kt - 4 * c + 12
                        pr.act(pt[:, off:TC], sb[:, off:TC], AF.Exp, bias=AB[:, h * 16 + d:h * 16 + d + 1], scale=0.125)
                        last = (kt == nkt - 1)
                        queue.append((ob, va, kt, pt, off, last, fin if last else None))
                        if len(queue) > LA:
                            emit_pv(queue.pop(0))
            while queue:
                emit_pv(queue.pop(0))
        gcol = pcol(l, "attn_out")
        for c in range(NCH):
            r = self.rms_stats([self.merged(mc, c) for mc in range(4)], 512, RMS_EPS)
            for mc in range(4):
                pr.stt(self.merged(mc, c), self.merged(mc, c), self.pp[:, gcol + mc:gcol + mc + 1], r, ALU.mult, ALU.mult)
        for c in range(NCH):
            for m in range(8):
                bank = self.banks[m % 4]
                for kc in range(KC):
                    pr.mm(bank, wo3[:, kc, m * 128:(m + 1) * 128], self.merged(kc, c), start=(kc == 0), stop=(kc == KC - 1))
                pr.act(self.xsM(m, c), bank, AF.Identity)
                if m == 3:
                    self.tick()
            self.final(c, lambda m, c=c: self.xsM(m, c), pcol(l, "mix_post"), False)
            self.tick()

    def ple(self, l, s):
        pr = self.P
        RW, RS = self.RW, self.RS
        r3 = lambda a, k: a.rearrange("p (k n) -> p k n", k=k)
        fs = self.ple_pre
        self.ple_pre = None
        pslot = 0 if fs is None else fs
        wpp = RW.tile(16384 + pslot * 4096, 4096)
        wpg_m = [RW.tile(m * 2048, 2048) for m in range(8)]
        early = [] if fs is None else list(range(4 * fs, 4 * fs + 4))
        morder = early + [m for m in range(8) if m not in early]
        for m in morder:
            if m not in early:
                pr.dma(r3(wpg_m[m].ap, 8), self.wpg[l, m], q="pool", writes=[wpg_m[m]])
        if fs is None:
            pr.dma(r3(wpp.ap, 2), self.wpp[l], q="pool", writes=[wpp])
        wpg3 = [w.v(lambda a: r3(a, 8)) for w in wpg_m]
        wpp3 = wpp.v(lambda a: r3(a, 2))
        def pt_t(c):
            return RS.tile((c % 2) * 2048, 2048)

        def load_pt(c):
            pt = pt_t(c)
            pr.dma(r3(pt.ap, 2), self.pT[l, s, :, :, c * TC:(c + 1) * TC], q="pool", writes=[pt])

        load_pt(0)
        load_pt(1)
        for c in range(NCH):
            self.need(c)
            pt3 = pt_t(c).v(lambda a: r3(a, 2))
            for mi, m in enumerate(morder):
                bg = self.banks[(mi % 2) * 2]
                bp = self.banks[(mi % 2) * 2 + 1]
                for kc in range(KC):
                    pr.mm(bg, wpg3[m][:, kc, :], self.hnF(kc, c), start=(kc == 0), stop=(kc == KC - 1))
                for k2 in range(2):
                    pr.mm(bp, wpp3[:, k2, m * 128:(m + 1) * 128], pt3[:, k2, :], start=(k2 == 0), stop=(k2 == 1))
                sg = RS.tile(4096 + (mi % 2) * 2048, 2048, F32)
                pr.act(sg, bg, AF.Sigmoid)
                pr.tt(self.acc(m, c), sg, bp, ALU.mult)
                if mi == 3:
                    self.tick()
            if c + 2 < NCH:
                load_pt(c + 2)
            self.final(c, lambda m, c=c: self.acc(m, c), pcol(l, "ple_post"), False)
            self.tick()


_CACHE = {}


def get_builder(**kw):
    key = tuple(sorted((k, str(v)) for k, v in kw.items()))
    if key not in _CACHE:
        _CACHE[key] = Builder(**kw)
    return _CACHE[key]


def _fm(v, nchunk):
    return np.ascontiguousarray(np.asarray(v, np.float32).reshape(nchunk, 128).T)


def _wk(w, kc):
    K, N = w.shape
    return np.ascontiguousarray(w.reshape(kc, 128, N).transpose(1, 0, 2))


def prep_shared(inp):
    f32 = np.float32
    sh = {}
    wg = np.empty((L * 2, NG, 128, 8, G * 128), f32)
    wu = np.empty_like(wg)
    wd = np.empty((L * 2, NG, 128, G, D), f32)
    for l in range(L):
        for which, nm in enumerate(("ffn1", "ffn2")):
            f = l * 2 + which
            g_ = np.asarray(inp[nm + "_w_gate"][l], f32)
            u_ = np.asarray(inp[nm + "_w_up"][l], f32)
            d_ = np.asarray(inp[nm + "_w_down"][l], f32)
            for gi in range(NG):
                cs = slice(gi * G * 128, (gi + 1) * G * 128)
                wg[f, gi] = _wk(g_[:, cs], 8)
                wu[f, gi] = _wk(u_[:, cs], 8)
                wd[f, gi] = d_[cs, :].reshape(G, 128, D).transpose(1, 0, 2)
    sh["wg"], sh["wu"], sh["wd"] = wg, wu, wd
    w_in = np.asarray(inp["w_in"], f32)
    sh["wvg"] = np.stack([np.stack([_wk(w_in[l][:, 1024:1536], 8), _wk(w_in[l][:, 2048:2560], 8)]) for l in range(L)])
    sh["wgu"] = np.stack([_wk(w_in[l][:, 1536:2048], 8) for l in range(L)])
    sh["wqk"] = np.stack([np.stack([_wk(np.concatenate([w_in[l][:, p * 128:(p + 1) * 128],
                                                        w_in[l][:, 512 + p * 128:512 + (p + 1) * 128]], 1), 8)
                                    for p in range(4)]) for l in range(L)])
    sh["wo"] = np.stack([_wk(np.asarray(inp["w_out"][l], f32), 8) for l in range(L)])
    sh["wpg"] = np.stack([np.ascontiguousarray(_wk(np.asarray(inp["ple_w_gate"][l], f32), 8).reshape(128, 8, 8, 128).transpose(2, 0, 1, 3))
                          for l in range(L)])
    sh["wpp"] = np.stack([_wk(np.asarray(inp["ple_w_proj"][l], f32), 2) for l in range(L)])
    ws = np.asarray(inp["gmlp_w_s"], f32)
    sh["wsT"] = np.ascontiguousarray(ws.transpose(0, 3, 1, 2).reshape(L, 128, 8 * 128))
    sh["bs"] = np.ascontiguousarray(np.asarray(inp["gmlp_b_s"], f32).reshape(L, 1, 1024))
    sh["lng"] = np.ascontiguousarray(np.broadcast_to(np.asarray(inp["gmlp_ln_g"], f32)[:, None, :], (L, 128, 512)))
    sh["lnb"] = np.ascontiguousarray(np.broadcast_to(np.asarray(inp["gmlp_ln_b"], f32)[:, None, :], (L, 128, 512)))
    pp = np.zeros((128, L * PL), f32)
    for l in range(L):
        for nm, key in (("ffn1_pre", "ffn1_pre_norm"), ("ffn1_post", "ffn1_post_norm"), ("mix_pre", "mix_pre_norm"),
                        ("mix_post", "mix_post_norm"), ("ffn2_pre", "ffn2_pre_norm"), ("ffn2_post", "ffn2_post_norm"),
                        ("ple_pre", "ple_pre_norm"), ("ple_post", "ple_post_norm")):
            pp[:, pcol(l, nm):pcol(l, nm) + 8] = _fm(inp[key][l], 8)
        pp[:, pcol(l, "attn_out"):pcol(l, "attn_out") + 4] = _fm(inp["attn_out_norm"][l], 4)
        pp[:, pcol(l, "gmlp_out"):pcol(l, "gmlp_out") + 4] = _fm(inp["gmlp_out_norm"][l], 4)
    sh["pp"] = pp
    i = np.arange(128)
    cb = np.zeros((128, 512), f32)
    cb[:, 0:128] = 1.0
    cb[:, 128:256] = np.eye(128, dtype=f32)
    cb[:, 256:384] = np.where(i[:, None] > i[None, :], -BIG, 0.0)
    cb[:, 384:512] = (i[:, None] <= i[None, :]).astype(f32)
    sh["cb"] = cb
    slopes = np.array([0.5 ** (h + 1) for h in range(8)], f32)
    cf = np.zeros((128, 256), f32)
    for h in range(8):
        for d in range(16):
            cf[:, h * 16 + d] = slopes[h] * (i + 128.0 * (d - 12))
    for b in range(8):
        for j in range(8):
            cf[:, 128 + b * 8 + j] = 0.0 if j < b else -1e30
            cf[:, 192 + b * 8 + j] = -BIG if j < b else 0.0
    sh["cf"] = cf
    t = np.arange(S)
    dl = t % 512
    qaug = np.zeros((8, 2, S), f32)
    for h in range(8):
        qaug[h, 0] = -8.0 * slopes[h] * (dl % 256)
        qaug[h, 1] = -8.0 * slopes[h] * 256.0 * (dl // 256)
    sh["qaug"] = qaug
    kaug = np.zeros((10, S), f32)
    for j in range(8):
        kaug[j] = (t // 256 == j)
    kaug[8:10] = 1.0
    sh["kaug"] = kaug
    return sh


def prep_core(inp, core, n_seq=NS):
    x = np.asarray(inp["x"], np.float32)
    p = np.asarray(inp["p"], np.float32)
    b0 = core * n_seq
    xT = np.stack([x[b0 + s].T.reshape(8, 128, S).transpose(1, 0, 2) for s in range(n_seq)])
    pT = np.stack([np.stack([p[l, b0 + s].T.reshape(2, 128, S).transpose(1, 0, 2) for s in range(n_seq)]) for l in range(L)])
    return {"xT": np.ascontiguousarray(xT), "pT": np.ascontiguousarray(pT)}


def kernel(**inputs):
    b = get_builder()
    sh = prep_shared(inputs)
    in_maps = []
    for core in range(N_CORES):
        m = dict(sh)
        m.update(prep_core(inputs, core))
        in_maps.append(m)
    res = run_bass_kernel_spmd(b.nc, in_maps, core_ids=list(range(N_CORES)))
    out = np.empty((16, S, D), np.float32)
    for core in range(N_CORES):
        oT = res.results[core]["outT"]
        for s in range(NS):
            out[core * NS + s] = oT[s].transpose(2, 1, 0).reshape(S, D)
    return out
```

```python
import math
from contextlib import ExitStack

import numpy as np
import concourse.bass as bass
import concourse.mybir as mybir
from concourse.bass_utils import run_bass_kernel_spmd

F32 = mybir.dt.float32
BF16 = mybir.dt.bfloat16
AF = mybir.ActivationFunctionType
ALU = mybir.AluOpType
AX = mybir.AxisListType

N_CORES = 8
L = 2
NS = 2
S = 2048
D = 1024
KC = 8
TC = 512
NCH = S // TC
DFF = 2816
G = 2
NG = DFF // (128 * G)
PL = 72
RMS_EPS = 1e-6
LN_EPS = 1e-5
BIG = 30000.0

ENGS = ["pe", "act", "dve", "pool", "sp"]
import os
SAME_ENG_SYNC = os.environ.get("MK_SES", "1") == "1"
RING = 4
DMA_RING = 8
RES_ENG = os.environ.get("MK_RES", "dve")
LNEXP = os.environ.get("MK_LNEXP", "1") == "1"
ACC_SPLIT = os.environ.get("MK_ACCSPLIT", "1") == "1"


class Buf:
    __slots__ = ("name", "writer", "rd_eng", "rd_dma")

    def __init__(self, name):
        self.name = name
        self.writer = None
        self.rd_eng = {}
        self.rd_dma = []


class Op:
    __slots__ = ("eng", "fn", "dma", "deps", "need_inc", "inc_no", "dma_no", "waits", "know")

    def __init__(self, eng, fn, dma):
        self.eng = eng
        self.fn = fn
        self.dma = dma
        self.deps = set()
        self.need_inc = False
        self.inc_no = 0
        self.dma_no = -1
        self.waits = []
        self.know = None


class T:
    __slots__ = ("ap", "bufs")

    def __init__(self, ap, bufs):
        self.ap = ap
        self.bufs = list(bufs)

    def __getitem__(self, k):
        return T(self.ap[k], self.bufs)

    def v(self, fn):
        return T(fn(self.ap), self.bufs)


class Region:
    def __init__(self, nc, es, name, nbytes, page=1024):
        self.t = es.enter_context(nc.sbuf_tensor(name, [128, nbytes // 2], BF16))
        self.page = page
        self.bufs = [Buf(f"{name}{i}") for i in range((nbytes + page - 1) // page)]

    def tile(self, off, nbytes, dtype=BF16):
        ap = self.t[:, off // 2:(off + nbytes) // 2]
        if dtype is F32:
            ap = ap.bitcast(F32)
        b0 = off // self.page
        b1 = (off + nbytes + self.page - 1) // self.page
        return T(ap, self.bufs[b0:b1])


class Prog:
    def __init__(self, nc):
        self.nc = nc
        self.ops = []
        self.eng_ops = {e: [] for e in ENGS}
        self.ndma = {"sp": 0, "pool": 0}

    def add(self, eng, fn, reads=(), writes=(), dma=False):
        op = Op(eng, fn, dma)
        deps = op.deps
        for t in reads:
            for b in t.bufs:
                if b.writer is not None:
                    deps.add(b.writer)
        for t in writes:
            for b in t.bufs:
                if b.writer is not None:
                    deps.add(b.writer)
                deps.update(b.rd_eng.values())
                deps.update(b.rd_dma)
        for t in reads:
            for b in t.bufs:
                if dma:
                    b.rd_dma.append(op)
                else:
                    b.rd_eng[eng] = op
        for t in writes:
            for b in t.bufs:
                b.writer = op
                b.rd_eng = {}
                b.rd_dma = []
        deps.discard(op)
        if dma:
            op.dma_no = self.ndma[eng]
            self.ndma[eng] += 1
        self.ops.append(op)
        self.eng_ops[eng].append(op)
        return op

    def mm(self, out, lhsT, rhs, start=True, stop=True):
        rd = [lhsT, rhs] + ([] if start else [out])
        return self.add("pe", lambda e: e.matmul(out.ap, lhsT.ap, rhs.ap, start=start, stop=stop), rd, [out])

    def act(self, out, in_, func, bias=0.0, scale=1.0, extra_reads=()):
        b = bias.ap if isinstance(bias, T) else bias
        rd = [in_] + ([bias] if isinstance(bias, T) else []) + list(extra_reads)
        return self.add("act", lambda e: e.activation(out=out.ap, in_=in_.ap, func=func, bias=b, scale=scale), rd, [out])

    def tt(self, out, in0, in1, op, eng="dve"):
        return self.add(eng, lambda e: e.tensor_tensor(out=out.ap, in0=in0.ap, in1=in1.ap, op=op), [in0, in1], [out])

    def stt(self, out, in0, scalar, in1, op0, op1, eng="dve"):
        s = scalar.ap if isinstance(scalar, T) else scalar
        rd = [in0, in1] + ([scalar] if isinstance(scalar, T) else [])
        return self.add(eng, lambda e: e.scalar_tensor_tensor(out=out.ap, in0=in0.ap, scalar=s, in1=in1.ap, op0=op0, op1=op1), rd, [out])

    def ts(self, out, in0, s1, s2, op0, op1=None, eng="dve"):
        if op1 is None:
            return self.add(eng, lambda e: e.tensor_scalar(out=out.ap, in0=in0.ap, scalar1=s1, scalar2=None, op0=op0), [in0], [out])
        return self.add(eng, lambda e: e.tensor_scalar(out=out.ap, in0=in0.ap, scalar1=s1, scalar2=s2, op0=op0, op1=op1), [in0], [out])

    def copy(self, out, in_, eng="dve"):
        return self.add(eng, lambda e: e.tensor_copy(out=out.ap, in_=in_.ap), [in_], [out])

    def recip(self, out, in_):
        return self.add("dve", lambda e: e.reciprocal(out=out.ap, in_=in_.ap), [in_], [out])

    def reduce(self, out, in_, op=ALU.add):
        return self.add("dve", lambda e: e.tensor_reduce(out=out.ap, in_=in_.ap, axis=AX.X, op=op), [in_], [out])

    def memset(self, out, val, eng="dve"):
        return self.add(eng, lambda e: e.memset(out.ap, val), [], [out])

    def dma(self, out, in_, q="sp", reads=(), writes=()):
        return self.add(q, lambda e: e.dma_start(out=out, in_=in_), reads, writes, dma=True)

    def wait_all(self, eng, ops):
        op = Op(eng, None, False)
        op.deps = set(ops)
        self.ops.append(op)
        self.eng_ops[eng].append(op)
        return op

    def finalize(self, es):
        nc = self.nc
        for op in self.ops:
            for d in op.deps:
                if d.dma:
                    continue
                if d.eng == op.eng and not op.dma and (op.eng == "pe" or not SAME_ENG_SYNC):
                    continue
                d.need_inc = True
        cnt = {e: 0 for e in ENGS}
        for op in self.ops:
            if op.need_inc:
                cnt[op.eng] += 1
                op.inc_no = cnt[op.eng]
        self.sems = {e: [es.enter_context(nc.semaphore(f"s_{e}{i}")) for i in range(RING)] for e in ENGS}
        self.dsems = {q: [es.enter_context(nc.semaphore(f"d_{q}{i}")) for i in range(DMA_RING)] for q in ("sp", "pool")}
        know = {e: {} for e in ENGS}
        know_dma = {e: set() for e in ENGS}
        nwaits = 0
        for op in self.ops:
            e = op.eng
            K = know[e]
            if op.dma and op.dma_no >= DMA_RING:
                n = op.dma_no - DMA_RING
                key = (e, n)
                if key not in know_dma[e]:
                    op.waits.append((self.dsems[e][n % DMA_RING], 16 * (n // DMA_RING + 1)))
                    know_dma[e].add(key)
            for d in sorted(op.deps, key=lambda o: (o.eng, o.inc_no, o.dma_no)):
                if d.dma:
                    key = (d.eng, d.dma_no)
                    if key in know_dma[e]:
                        continue
                    op.waits.append((self.dsems[d.eng][d.dma_no % DMA_RING], 16 * (d.dma_no // DMA_RING + 1)))
                    know_dma[e].add(key)
                    continue
                if d.eng == e and not op.dma and (e == "pe" or not SAME_ENG_SYNC):
                    continue
                if K.get(d.eng, 0) >= d.inc_no:
                    continue
                n = d.inc_no - 1
                op.waits.append((self.sems[d.eng][n % RING], n // RING + 1))
                for k2, v2 in d.know.items():
                    if K.get(k2, 0) < v2:
                        K[k2] = v2
            nwaits += len(op.waits)
            if op.need_inc:
                kk = dict(K)
                kk[e] = max(kk.get(e, 0), op.inc_no)
                op.know = kk
        self.nwaits = nwaits

    def emit(self, es):
        nc = self.nc
        block = es.enter_context(nc.Block())
        prog = self

        def run(name, e):
            sems = prog.sems[name]
            with nc.allow_low_precision("bf16 matmul operands, fp32 accumulation"):
                for op in prog.eng_ops[name]:
                    for (sem, val) in op.waits:
                        e.wait_ge(sem, val)
                    if op.fn is None:
                        continue
                    ins = op.fn(e)
                    if op.dma:
                        ins.then_inc(prog.dsems[name][op.dma_no % DMA_RING], 16)
                    elif op.need_inc:
                        ins.then_inc(sems[(op.inc_no - 1) % RING], 1)

        @block.tensor
        def _(e):
            run("pe", e)

        @block.scalar
        def _(e):
            run("act", e)

        @block.vector
        def _(e):
            run("dve", e)

        @block.gpsimd
        def _(e):
            run("pool", e)

        @block.sync
        def _(e):
            run("sp", e)


def pcol(l, name):
    base = l * PL
    return base + {"ffn1_pre": 0, "ffn1_post": 8, "mix_pre": 16, "mix_post": 24, "ffn2_pre": 32, "ffn2_post": 40,
                   "ple_pre": 48, "ple_post": 56, "attn_out": 64, "gmlp_out": 68}[name]


class Builder:
    def __init__(self, n_seq=NS, n_layers=L, stages=("ffn1", "mix", "ffn2", "ple")):
        self.n_seq = n_seq
        self.n_layers = n_layers
        self.stages = stages
        self.nc = bass.Bass("TRN2", target_bir_lowering=False)
        self.es = ExitStack()
        self.P = Prog(self.nc)
        self.declare()
        self.alloc()
        self.program()
        self.P.finalize(self.es)
        self.P.emit(self.es)
        self.es.close()

    def declare(self):
        nc = self.nc
        ns = self.n_seq

        def inp(name, shape):
            return nc.dram_tensor(name, list(shape), F32, kind="ExternalInput").ap()

        self.xT = inp("xT", [ns, 128, 8, S])
        self.pT = inp("pT", [L, ns, 128, 2, S])
        self.wg = inp("wg", [L * 2, NG, 128, 8, G * 128])
        self.wu = inp("wu", [L * 2, NG, 128, 8, G * 128])
        self.wd = inp("wd", [L * 2, NG, 128, G, D])
        self.wvg = inp("wvg", [L, 2, 128, 8, 512])
        self.wgu = inp("wgu", [L, 128, 8, 512])
        self.wqk = inp("wqk", [L, 4, 128, 8, 256])
        self.wo = inp("wo", [L, 128, 8, 1024])
        self.wpg = inp("wpg", [L, 8, 128, 8, 128])
        self.wpp = inp("wpp", [L, 128, 2, 1024])
        self.wsT = inp("wsT", [L, 128, 8 * 128])
        self.bsd = inp("bs", [L, 1, 1024])
        self.lng = inp("lng", [L, 128, 512])
        self.lnb = inp("lnb", [L, 128, 512])
        self.ppd = inp("pp", [128, L * PL])
        self.cbd = inp("cb", [128, 512])
        self.cfd = inp("cf", [128, 256])
        self.qaugd = inp("qaug", [8, 2, S])
        self.kaugd = inp("kaug", [10, S])
        self.outT = nc.dram_tensor("outT", [ns, 128, 8, S], F32, kind="ExternalOutput").ap()

    def alloc(self):
        nc, es = self.nc, self.es
        self.RH = Region(nc, es, "hT", 65536, page=2048)
        self.RB = Region(nc, es, "big", 65536)
        self.RC = Region(nc, es, "rc", 32768)
        self.RW = Region(nc, es, "rw", 24576)
        self.RS = Region(nc, es, "rs", 16384)
        self.RK = Region(nc, es, "rk", 7168, page=256)
        self.banks = []
        for i in range(8):
            t = es.enter_context(nc.psum_tensor(f"ps{i}", [128, 512], F32))
            self.banks.append(T(t[:, :], [Buf(f"ps{i}")]))
        RK = self.RK
        self.ones = RK.tile(0, 256)
        self.ident = RK.tile(256, 256)
        self.trineg = RK.tile(512, 256)
        self.causT = RK.tile(768, 256)
        self.cf = RK.tile(1024, 1024, F32)
        self.pp = RK.tile(2048, 1024, F32)
        self.pph = RK.tile(3072, 1024, F32)

    def h(self, m, c):
        return self.RH.tile((m * S + c * TC) * 4, TC * 4, F32)

    def acc(self, m, c):
        return self.RB.tile((c * 8 + m) * 2048, 2048, F32)

    def hnF(self, kc, c):
        return self.RC.tile((c * 8 + kc) * 1024, 1024)

    HN_SLOT = (0, 1, 3, 5)
    MG_SLOT = (2, 4, 6, 7)

    def hnM(self, kc, c):
        return self.RB.tile(self.HN_SLOT[c] * 8192 + kc * 1024, 1024)

    def merged(self, kc, c):
        return self.RB.tile(self.MG_SLOT[c] * 8192 + kc * 1024, 1024)

    def xsM(self, m, c):
        if c % 2 == 0:
            return self.RB.tile(m * 2048, 2048, F32)
        slot = 3 if m < 4 else 5
        return self.RB.tile(slot * 8192 + (m % 4) * 2048, 2048, F32)

    def hview(self, c):
        return T(self.RH.t[:, :].bitcast(F32).rearrange("p (m t) -> p m t", m=8)[:, :, c * TC:(c + 1) * TC],
                 [b for m in range(8) for b in self.h(m, c).bufs])

    def program(self):
        P = self.P
        P.dma(self.RK.t[:, 0:512], self.cbd, q="pool", writes=[self.ones, self.ident, self.trineg, self.causT])
        P.dma(self.cf.ap, self.cfd, q="sp", writes=[self.cf])
        P.dma(self.pp.ap[:, 0:L * PL], self.ppd, q="sp", writes=[self.pp])
        P.ts(self.pph[:, 0:L * PL], self.pp[:, 0:L * PL], 0.5, None, ALU.mult)
        self.cnt = 0
        self.dcnt = 0
        self.rcnt = 0
        self.tickno = 0
        self.pending = []
        self.out_dmas = []
        self.ffn_base = 0
        self.mix_pre = None
        self.ple_pre = None
        self.early_hook = None
        for s in range(self.n_seq):
            blocks = []
            for l in range(self.n_layers):
                for st in ("ffn1", "mix", "ffn2", "ple"):
                    if st in self.stages:
                        blocks.append((st, l))
            self.cur_s = s
            self.set_next(blocks, -1)
            for c in range(NCH):
                self.final(c, None, None, False, start=True)
                self.tick()
            for bi, (st, l) in enumerate(blocks):
                self.set_next(blocks, bi)
                self.early_hook = None
                if st in ("ffn1", "ffn2") and bi + 1 < len(blocks) and blocks[bi + 1][0] in ("mix", "ple"):
                    self.early_hook = (lambda fslot, nb=blocks[bi + 1]: self.early_load(nb[0], nb[1], fslot))
                if st == "ffn1":
                    self.ffn(l, 0)
                elif st == "mix":
                    self.mixer(l, s)
                elif st == "ffn2":
                    self.ffn(l, 1)
                else:
                    self.ple(l, s)
            self.flush()
        P.wait_all("sp", self.out_dmas)

    def set_next(self, blocks, bi):
        if bi + 1 < len(blocks):
            st, l = blocks[bi + 1]
            name = {"ffn1": "ffn1_pre", "mix": "mix_pre", "ffn2": "ffn2_pre", "ple": "ple_pre"}[st]
            self.next_pre = (pcol(l, name), self.hnM if st == "mix" else self.hnF)
        else:
            self.next_pre = None
        self.sq_ring_mode = False
        self.next_is_mix = (bi + 1 < len(blocks) and blocks[bi + 1][0] == "mix")

    def dbank(self):
        b = self.banks[4 + self.dcnt % 4]
        self.dcnt += 1
        return b

    def sq_t(self, i):
        return self.RS.tile(14336 + (i % 2) * 1024, 1024)

    def rstd_t(self):
        self.rcnt += 1
        return self.RS.tile(8192 + (self.rcnt % 2) * 2048, 2048, F32)

    def rms_stats(self, tiles, nfeat, eps):
        pr = self.P
        bank = self.dbank()
        n = len(tiles)
        for i, t in enumerate(tiles):
            sq = self.sq_t(self.cnt)
            self.cnt += 1
            pr.act(sq, t, AF.Square)
            pr.mm(bank, self.ones, sq, start=(i == 0), stop=(i == n - 1))
        r = self.rstd_t()
        if LNEXP:
            pr.act(r, bank, AF.Ln, bias=float(eps), scale=1.0 / nfeat)
            pr.act(r, r, AF.Exp, scale=-0.5)
        else:
            pr.act(r, bank, AF.Sqrt, bias=float(eps), scale=1.0 / nfeat)
            pr.recip(r, r)
        return r

    def tick(self):
        self.tickno += 1
        keep = []
        for item in self.pending:
            if item[0] <= self.tickno:
                item[2]()
            else:
                keep.append(item)
        self.pending = keep

    def need(self, c):
        keep = []
        for item in self.pending:
            if item[1] <= c:
                item[2]()
            else:
                keep.append(item)
        self.pending = keep

    def flush(self):
        for item in self.pending:
            item[2]()
        self.pending = []

    def stats_from(self, sqs):
        pr = self.P
        bank = self.dbank()
        for m in range(8):
            pr.mm(bank, self.ones, sqs[m], start=(m == 0), stop=(m == 7))
        r = self.rstd_t()
        if LNEXP:
            pr.act(r, bank, AF.Ln, bias=float(RMS_EPS), scale=1.0 / D)
            pr.act(r, r, AF.Exp, scale=-0.5)
        else:
            pr.act(r, bank, AF.Sqrt, bias=float(RMS_EPS), scale=1.0 / D)
            pr.recip(r, r)
        return r

    def final(self, c, src, gcol_post, half, start=False):
        pr = self.P
        nxt = self.next_pre
        ring = self.next_is_mix and c == 3
        sqb = [self.hnF(m, c) for m in range(8)]
        s = self.cur_s

        def squares(tiles):
            for m in range(8):
                pr.act(sqb[m], tiles[m], AF.Square)

        def pre_part():
            gcol, dst = nxt
            if ring:
                r = self.rms_stats([self.h(m, c) for m in range(8)], D, RMS_EPS)
            else:
                r = self.stats_from(sqb)
            for m in range(8):
                pr.stt(dst(m, c), self.h(m, c), self.pp[:, gcol + m:gcol + m + 1], r, ALU.mult, ALU.mult)

        def post_part():
            srcs = [src(m) for m in range(8)]
            if ring:
                r = self.rms_stats(srcs, D, RMS_EPS)
            else:
                r = self.stats_from(sqb)
            gp = self.pph if half else self.pp
            for m in range(8):
                pr.stt(srcs[m], srcs[m], gp[:, gcol_post + m:gcol_post + m + 1], r, ALU.mult, ALU.mult)
            for m in range(8):
                pr.tt(self.h(m, c), self.h(m, c), srcs[m], ALU.add, eng=RES_ENG)
            if nxt is not None:
                if not ring:
                    squares([self.h(m, c) for m in range(8)])
            else:
                hv = self.hview(c)
                self.out_dmas.append(pr.dma(self.outT[s, :, :, c * TC:(c + 1) * TC], hv.ap, q="sp", reads=[hv]))

        if start:
            hv = self.hview(c)
            pr.dma(hv.ap, self.xT[s, :, :, c * TC:(c + 1) * TC], q="sp", writes=[hv])
            if nxt is not None:
                squares([self.h(m, c) for m in range(8)])
                self.pending.append((self.tickno + 2, c, pre_part))
            return
        if not ring:
            squares([src(m) for m in range(8)])
        self.pending.append((self.tickno + 2, c, post_part))
        if nxt is not None:
            self.pending.append((self.tickno + 4, c, pre_part))

    def early_load(self, st, l, fslot):
        pr = self.P
        RW = self.RW
        r3 = lambda a, k: a.rearrange("p (k n) -> p k n", k=k)
        if st == "mix":
            wv = RW.tile(fslot * 8192, 8192)
            pr.dma(r3(wv.ap, 8), self.wvg[l, 0], q="pool", writes=[wv])
            self.mix_pre = fslot
        else:
            for m in range(4 * fslot, 4 * fslot + 4):
                w = RW.tile(m * 2048, 2048)
                pr.dma(r3(w.ap, 8), self.wpg[l, m], q="pool", writes=[w])
            wpp = RW.tile(16384 + fslot * 4096, 4096)
            pr.dma(r3(wpp.ap, 2), self.wpp[l], q="pool", writes=[wpp])
            self.ple_pre = fslot

    def ffn(self, l, which):
        pr = self.P
        f = l * 2 + which
        post = pcol(l, "ffn1_post" if which == 0 else "ffn2_post")
        RW, RS = self.RW, self.RS
        base = self.ffn_base

        def wgu_t(gi):
            return RW.tile(((gi + base) % 2) * 8192, 8192)

        def wd_t(gi):
            return RW.tile(16384 + ((gi + base) % 2) * 4096, 4096)

        def load(gi):
            w = wgu_t(gi)
            v = w.ap.rearrange("p (a k n) -> p a k n", a=2, k=8)
            pr.dma(v[:, 0], self.wg[f, gi], q="pool", writes=[w])
            pr.dma(v[:, 1], self.wu[f, gi], q="pool", writes=[w])
            wdt = wd_t(gi)
            pr.dma(wdt.ap.rearrange("p (j n) -> p j n", j=G), self.wd[f, gi], q="pool", writes=[wdt])

        load(0)
        steps = [(gi, c) for gi in range(NG) for c in range(NCH)]
        hid = 0

        def sg_t(i):
            return RS.tile((i % 2) * 2048, 2048, F32)

        def act_t(i, jj):
            return RS.tile(4096 + (i % 2) * 2048 + jj * 1024, 1024)

        def GU(i):
            nonlocal hid
            gi, c = steps[i]
            w = wgu_t(gi).v(lambda a: a.rearrange("p (a k n) -> p a k n", a=2, k=8))
            for jj in range(G):
                bg = self.banks[(hid % 2) * 2]
                bu = self.banks[(hid % 2) * 2 + 1]
                for kc in range(KC):
                    pr.mm(bg, w[:, 0, kc, jj * 128:(jj + 1) * 128], self.hnF(kc, c), start=(kc == 0), stop=(kc == KC - 1))
                for kc in range(KC):
                    pr.mm(bu, w[:, 1, kc, jj * 128:(jj + 1) * 128], self.hnF(kc, c), start=(kc == 0), stop=(kc == KC - 1))
                sg = sg_t(hid)
                pr.act(sg, bg, AF.Silu)
                pr.tt(act_t(i, jj), sg, bu, ALU.mult)
                hid += 1

        def Dn(i):
            gi, c = steps[i]
            if gi == 0 and c == 0:
                self.need(NCH - 1)
            wdt = wd_t(gi).v(lambda a: a.rearrange("p (j n) -> p j n", j=G))
            for m in range(8):
                bank = self.dbank()
                for jj in range(G):
                    pr.mm(bank, wdt[:, jj, m * 128:(m + 1) * 128], act_t(i, jj), start=(jj == 0), stop=(jj == G - 1))
                if ACC_SPLIT and m % 2 == 1:
                    if gi == 0:
                        pr.act(self.acc(m, c), bank, AF.Identity)
                    else:
                        tmp = RS.tile(12288 + (self.cnt % 2) * 2048, 2048, F32)
                        self.cnt += 1
                        pr.act(tmp, bank, AF.Identity)
                        pr.tt(self.acc(m, c), tmp, self.acc(m, c), ALU.add, eng="pool")
                elif gi == 0:
                    pr.copy(self.acc(m, c), bank)
                else:
                    pr.tt(self.acc(m, c), bank, self.acc(m, c), ALU.add)
            if gi == NG - 1:
                self.final(c, lambda m, c=c: self.acc(m, c), post, True)

        for i in range(len(steps)):
            gi, c = steps[i]
            if gi == 0:
                self.need(c)
            GU(i)
            self.tick()
            if i >= 1:
                Dn(i - 1)
            if c == 0 and gi + 1 < NG:
                load(gi + 1)
            if c == 0 and gi == NG - 1 and self.early_hook is not None:
                self.early_hook((NG - 2 + base) % 2)
            self.tick()
        Dn(len(steps) - 1)
        self.ffn_base = (base + NG) % 2

    def mixer(self, l, s):
        pr = self.P
        RW, RS, RC, RB, RK = self.RW, self.RS, self.RC, self.RB, self.RK
        r3 = lambda a, k: a.rearrange("p (k n) -> p k n", k=k)
        fs = self.mix_pre
        self.mix_pre = None
        vslot = 0 if fs is None else fs
        wv = RW.tile(vslot * 8192, 8192)
        wgv = RW.tile((1 - vslot) * 8192, 8192)
        wgu = RW.tile(16384, 8192)
        if fs is None:
            pr.dma(r3(wv.ap, 8), self.wvg[l, 0], q="pool", writes=[wv])
        pr.dma(r3(wgv.ap, 8), self.wvg[l, 1], q="pool", writes=[wgv])
        pr.dma(r3(wgu.ap, 8), self.wgu[l], q="pool", writes=[wgu])
        wv3 = wv.v(lambda a: r3(a, 8))
        wgv3 = wgv.v(lambda a: r3(a, 8))
        wgu3 = wgu.v(lambda a: r3(a, 8))
        lng = RC.tile(24576, 2048, F32)
        lnb = RC.tile(26624, 2048, F32)
        wst = RC.tile(28672, 2048)
        bsb = RC.tile(30720, 2048)
        pr.dma(lng.ap, self.lng[l], q="sp", writes=[lng])
        pr.dma(lnb.ap, self.lnb[l], q="sp", writes=[lnb])
        pr.dma(wst.ap, self.wsT[l], q="pool", writes=[wst])
        pr.dma(bsb.ap[0:1, :], self.bsd[l], q="pool", writes=[bsb])
        wst3 = wst.v(lambda a: r3(a, 8))
        pr.tt(wst3, wst3, self.causT.v(lambda a: a.unsqueeze(1).to_broadcast([128, 8, 128])), ALU.mult)

        def Vt(t):
            return RC.tile(t * 1024, 1024)

        def vn_t(tt):
            return RS.tile(tt * 1024, 1024)

        def ge_t(i):
            return RS.tile(4096 + (i % 2) * 2048, 2048, F32)

        def small(i, k):
            return RK.tile(6144 + ((i % 4) * 8 + k) * 32, 32, F32)

        u_t = RK.tile(4096, 2048, F32)
        g3 = lambda a: a.rearrange("p (g d) -> p g d", g=8)
        bc3 = lambda a: a.unsqueeze(2).to_broadcast([128, 8, 64])

        def ge4(tt):
            return RS.tile(4096 + tt * 2048, 2048, F32)

        def ub(mc):
            return RS.tile(12288 + mc * 1024, 1024) if mc < 2 else RK.tile(4096 + (mc - 2) * 1024, 1024)

        s2all = RK.tile(6144 + 512, 128, F32)
        rsall = RK.tile(6144 + 640, 128, F32)
        for c in range(NCH):
            self.need(c)
            for tt in range(4):
                t = 4 * c + tt
                bv = self.banks[tt % 2]
                bg = self.banks[2 + tt % 2]
                for kc in range(KC):
                    pr.mm(bv, self.hnM(kc, c)[:, tt * 128:(tt + 1) * 128], wv3[:, kc, :], start=(kc == 0), stop=(kc == KC - 1))
                for kc in range(KC):
                    pr.mm(bg, self.hnM(kc, c)[:, tt * 128:(tt + 1) * 128], wgv3[:, kc, :], start=(kc == 0), stop=(kc == KC - 1))
                pr.act(Vt(t), bv, AF.Identity)
                ge = ge4(tt)
                pr.act(ge, bg, AF.Gelu)
                s1, mean = small(tt, 0), small(tt, 1)
                ge3 = ge.v(g3)
                pr.reduce(s1, ge3)
                pr.ts(mean, s1, 1.0 / 64, None, ALU.mult)
                pr.tt(ge3, ge3, mean.v(bc3), ALU.subtract)
            for mc in range(4):
                bu = self.banks[4 + mc]
                for kc in range(KC):
                    pr.mm(bu, wgu3[:, kc, mc * 128:(mc + 1) * 128], self.hnM(kc, c), start=(kc == 0), stop=(kc == KC - 1))
            for mc in range(4):
                pr.act(ub(mc), self.banks[4 + mc], AF.Gelu)
            for tt in range(4):
                sqb = self.sq_t(self.cnt)
                self.cnt += 1
                pr.act(sqb, ge4(tt), AF.Square)
                pr.reduce(s2all[:, tt * 8:(tt + 1) * 8], sqb.v(g3))
            pr.act(rsall, s2all, AF.Sqrt, bias=float(LN_EPS), scale=1.0 / 64)
            pr.recip(rsall, rsall)
            for tt in range(4):
                ge = ge4(tt)
                ge3 = ge.v(g3)
                pr.tt(ge3, ge3, rsall[:, tt * 8:(tt + 1) * 8].v(bc3), ALU.mult)
                pr.tt(ge, ge, lng, ALU.mult)
                pr.tt(vn_t(tt), ge, lnb, ALU.add)
            self.need(min(c + 1, NCH - 1))
            for mc in range(4):
                bsp = self.banks[mc]
                for e in range(2):
                    g = 2 * mc + e
                    for tt in range(4):
                        o = bsp[e * 64:(e + 1) * 64, tt * 128:(tt + 1) * 128]
                        pr.mm(o, vn_t(tt)[:, g * 64:(g + 1) * 64], wst[:, g * 128:(g + 1) * 128], start=True, stop=False)
                        pr.mm(o, self.ones[0:1, 0:64], bsb[0:1, g * 128:(g + 1) * 128], start=False, stop=True)
                pr.tt(self.merged(4 + mc, c), ub(mc), bsp, ALU.mult)
            r = self.rms_stats([self.merged(4 + mc, c) for mc in range(4)], 512, RMS_EPS)
            gcol = pcol(l, "gmlp_out")
            for mc in range(4):
                pr.stt(self.merged(4 + mc, c), self.merged(4 + mc, c), self.pp[:, gcol + mc:gcol + mc + 1], r, ALU.mult, ALU.mult)
            self.tick()
        self.flush()

        def wqk_t(p):
            return RW.tile(16384 + (p % 2) * 4096, 4096)

        def load_qk(p):
            w = wqk_t(p)
            pr.dma(r3(w.ap, 8), self.wqk[l, p], q="pool", writes=[w])

        wo = RW.tile(0, 16384)

        def qa_c(sl, c):
            return RS.tile(sl * 4096 + c * 1024, 1024)

        def ka_c(sl, c):
            return RS.tile(8192 + sl * 4096 + c * 1024, 1024)

        def qa_all(sl):
            return RS.tile(sl * 4096, 4096)

        def ka_all(sl):
            return RS.tile(8192 + sl * 4096, 4096)

        def Vaug(sl):
            return RC.tile(16384 + sl * 4096, 4096)

        def PT(i):
            return RC.tile(24576 + (i % 4) * 1024, 1024)

        rec = RC.tile(28672, 2048, F32)
        SM = 30720

        def gm_t(i):
            return RC.tile(SM + i * 32, 32, F32)

        def mx_t(i):
            return RC.tile(SM + 512 + i * 32, 32, F32)

        def pen_t(i):
            return RC.tile(SM + 1024 + i * 16, 16)

        def ksum_t(sl):
            return RC.tile(SM + 1280 + sl * 32, 32, F32)

        def kmean_t(sl):
            return RC.tile(SM + 1344 + sl * 16, 16)

        load_qk(0)
        load_qk(1)
        pr.dma(r3(wo.ap, 8), self.wo[l], q="pool", writes=[wo])
        wo3 = wo.v(lambda a: r3(a, 8))
        for sl in range(2):
            ka = ka_all(sl)
            pr.dma(ka.ap[64:74, :], self.kaugd, q="pool", writes=[ka])
            pr.memset(qa_all(sl)[64:72, 0:1024], 0.0)
            va = Vaug(sl).v(lambda a: a.rearrange("p (t n) -> p t n", t=16))
            if sl == 0:
                pr.memset(va[:, :, 64:128], 1.0)
            else:
                pr.memset(va[:, :, 0:64], 1.0)
        AB = self.cf
        scnt = 0
        ocnt = 0
        pcnt = 0
        Vall = T(RC.t[:, 0:8192].rearrange("p (t n) -> p t n", t=16), RC.bufs[0:16])
        for pair in range(4):
            w3 = wqk_t(pair).v(lambda a: r3(a, 8))
            for sl in range(2):
                pr.dma(qa_all(sl).ap[72:74, :], self.qaugd[2 * pair + sl], q="pool", writes=[qa_all(sl)])
            for c in range(NCH):
                bq = self.banks[5 if c % 2 == 0 else 3]
                bk = self.banks[6 if c % 2 == 0 else 4]
                for kc in range(KC):
                    pr.mm(bq, w3[:, kc, 0:128], self.hnM(kc, c), start=(kc == 0), stop=(kc == KC - 1))
                for kc in range(KC):
                    pr.mm(bk, w3[:, kc, 128:256], self.hnM(kc, c), start=(kc == 0), stop=(kc == KC - 1))
                pr.act(qa_c(0, c)[0:64, :], bq[0:64, :], AF.Identity)
                pr.copy(qa_c(1, c)[0:64, :], bq[64:128, :])
                pr.act(ka_c(0, c)[0:64, :], bk[0:64, :], AF.Identity)
                pr.copy(ka_c(1, c)[0:64, :], bk[64:128, :])
                pr.reduce(ksum_t(0)[0:64, 2 * c:2 * c + 2], bk[0:64, :].v(lambda a: a.rearrange("p (b n) -> p b n", b=2)))
                pr.reduce(ksum_t(1)[0:64, 2 * c:2 * c + 2], bk[64:128, :].v(lambda a: a.rearrange("p (b n) -> p b n", b=2)))
            if pair + 2 < 4:
                load_qk(pair + 2)
            gb = self.banks[7]
            for sl in range(2):
                pr.ts(kmean_t(sl)[0:64, :], ksum_t(sl)[0:64, :], 1.0 / 256, None, ALU.mult)
            for sl in range(2):
                for t in range(8, 16):
                    i = sl * 8 + t - 8
                    pr.mm(gb[:, i * 8:(i + 1) * 8], qa_c(sl, t // 4)[0:64, (t % 4) * 128:(t % 4 + 1) * 128], kmean_t(sl)[0:64, :])
            for sl in range(2):
                for t in range(8, 16):
                    i = sl * 8 + t - 8
                    b = t // 2
                    pr.tt(gm_t(i), gb[:, i * 8:(i + 1) * 8], self.cf[:, 128 + b * 8:128 + (b + 1) * 8], ALU.add)
            for i in range(16):
                gm, mx = gm_t(i), mx_t(i)
                pr.add("dve", (lambda gm=gm, mx=mx: (lambda e: e.max(out=mx.ap, in_=gm.ap)))(), [gm], [mx])
            for sl in range(2):
                for t in range(8, 16):
                    i = sl * 8 + t - 8
                    b = t // 2
                    pr.stt(pen_t(i), gm_t(i), mx_t(i)[:, 2:3], self.cf[:, 192 + b * 8:192 + (b + 1) * 8], ALU.is_lt, ALU.mult)

            def pen_rows():
                for sl in range(2):
                    for r in range(2):
                        tb = self.banks[5 + r]
                        for t4 in range(4):
                            t = 8 + 4 * r + t4
                            pr.mm(tb[64:72, t4 * 128:(t4 + 1) * 128], pen_t(sl * 8 + t - 8), self.ident)
                        pr.copy(qa_c(sl, 2 + r)[64:72, :], tb[64:72, :])

            for sl in range(2):
                h = 2 * pair + sl
                va = Vaug(sl).v(lambda a: a.rearrange("p (t n) -> p t n", t=16))
                dst = va[:, :, 0:64] if sl == 0 else va[:, :, 64:128]
                pr.copy(dst, Vall[:, :, h * 64:(h + 1) * 64])
            LA = 3
            queue = []

            def emit_pv(item):
                ob, va, pkt, ppt, poff, last, fin = item
                pr.mm(ob[:, poff:TC], va[:, pkt, :], ppt[:, poff:TC], start=(pkt == 0), stop=last)
                if fin is not None:
                    fin()

            for sl, c in [(0, 0), (0, 1), (1, 0), (1, 1), ("pen", None), (0, 2), (0, 3), (1, 2), (1, 3)]:
                if sl == "pen":
                    pen_rows()
                    continue
                if True:
                    h = 2 * pair + sl
                    va = Vaug(sl).v(lambda a: a.rearrange("p (t n) -> p t n", t=16))
                    nkt = 4 * c + 4
                    ob = self.banks[3 + ocnt % 2]
                    ocnt += 1
                    qc = qa_c(sl, c)
                    mt = self.merged(pair, c)

                    def fin(ob=ob, mt=mt, sl=sl):
                        if sl == 0:
                            pr.recip(rec[0:64, :], ob[64:128, :])
                            pr.tt(mt[0:64, :], ob[0:64, :], rec[0:64, :], ALU.mult)
                        else:
                            pr.recip(rec[64:128, :], ob[0:64, :])
                            pr.tt(mt[64:128, :], ob[64:128, :], rec[64:128, :], ALU.mult)

                    for kt in range(nkt):
                        sb = self.banks[(0, 1, 2, 7)[scnt % 4]]
                        scnt += 1
                        kk = ka_c(sl, kt // 4)[0:74, (kt % 4) * 128:(kt % 4 + 1) * 128]
                        diag = kt >= 4 * c
                        off = (kt - 4 * c) * 128 if diag else 0
                        if not diag:
                            pr.mm(sb, kk, qc[0:74, :])
                        else:
                            pr.mm(sb[:, off:off + 128], kk, qc[0:74, off:off + 128], start=True, stop=False)
                            pr.mm(sb[:, off:off + 128], self.ident, self.trineg, start=False, stop=True)
                            if off + 128 < TC:
                                pr.mm(sb[:, off + 128:TC], kk, qc[0:74, off + 128:TC])
                        pt = PT(pcnt)
                        pcnt += 1
                        d = kt - 4 * c + 12
                        pr.act(pt[:, off:TC], sb[:, off:TC], AF.Exp, bias=AB[:, h * 16 + d:h * 16 + d + 1], scale=0.125)
                        last = (kt == nkt - 1)
                        queue.append((ob, va, kt, pt, off, last, fin if last else None))
                        if len(queue) > LA:
                            emit_pv(queue.pop(0))
            while queue:
                emit_pv(queue.pop(0))
        gcol = pcol(l, "attn_out")
        for c in range(NCH):
            r = self.rms_stats([self.merged(mc, c) for mc in range(4)], 512, RMS_EPS)
            for mc in range(4):
                pr.stt(self.merged(mc, c), self.merged(mc, c), self.pp[:, gcol + mc:gcol + mc + 1], r, ALU.mult, ALU.mult)
        for c in range(NCH):
            for m in range(8):
                bank = self.banks[m % 4]
                for kc in range(KC):
                    pr.mm(bank, wo3[:, kc, m * 128:(m + 1) * 128], self.merged(kc, c), start=(kc == 0), stop=(kc == KC - 1))
                pr.act(self.xsM(m, c), bank, AF.Identity)
                if m == 3:
                    self.tick()
            self.final(c, lambda m, c=c: self.xsM(m, c), pcol(l, "mix_post"), False)
            self.tick()

    def ple(self, l, s):
        pr = self.P
        RW, RS = self.RW, self.RS
        r3 = lambda a, k: a.rearrange("p (k n) -> p k n", k=k)
        fs = self.ple_pre
        self.ple_pre = None
        pslot = 0 if fs is None else fs
        wpp = RW.tile(16384 + pslot * 4096, 4096)
        wpg_m = [RW.tile(m * 2048, 2048) for m in range(8)]
        early = [] if fs is None else list(range(4 * fs, 4 * fs + 4))
        morder = early + [m for m in range(8) if m not in early]
        for m in morder:
            if m not in early:
                pr.dma(r3(wpg_m[m].ap, 8), self.wpg[l, m], q="pool", writes=[wpg_m[m]])
        if fs is None:
            pr.dma(r3(wpp.ap, 2), self.wpp[l], q="pool", writes=[wpp])
        wpg3 = [w.v(lambda a: r3(a, 8)) for w in wpg_m]
        wpp3 = wpp.v(lambda a: r3(a, 2))
        def pt_t(c):
            return RS.tile((c % 2) * 2048, 2048)

        def load_pt(c):
            pt = pt_t(c)
            pr.dma(r3(pt.ap, 2), self.pT[l, s, :, :, c * TC:(c + 1) * TC], q="pool", writes=[pt])

        load_pt(0)
        load_pt(1)
        for c in range(NCH):
            self.need(c)
            pt3 = pt_t(c).v(lambda a: r3(a, 2))
            for mi, m in enumerate(morder):
                bg = self.banks[(mi % 2) * 2]
                bp = self.banks[(mi % 2) * 2 + 1]
                for kc in range(KC):
                    pr.mm(bg, wpg3[m][:, kc, :], self.hnF(kc, c), start=(kc == 0), stop=(kc == KC - 1))
                for k2 in range(2):
                    pr.mm(bp, wpp3[:, k2, m * 128:(m + 1) * 128], pt3[:, k2, :], start=(k2 == 0), stop=(k2 == 1))
                sg = RS.tile(4096 + (mi % 2) * 2048, 2048, F32)
                pr.act(sg, bg, AF.Sigmoid)
                pr.tt(self.acc(m, c), sg, bp, ALU.mult)
                if mi == 3:
                    self.tick()
            if c + 2 < NCH:
                load_pt(c + 2)
            self.final(c, lambda m, c=c: self.acc(m, c), pcol(l, "ple_post"), False)
            self.tick()


_CACHE = {}


def get_builder(**kw):
    key = tuple(sorted((k, str(v)) for k, v in kw.items()))
    if key not in _CACHE:
        _CACHE[key] = Builder(**kw)
    return _CACHE[key]


def _fm(v, nchunk):
    return np.ascontiguousarray(np.asarray(v, np.float32).reshape(nchunk, 128).T)


def _wk(w, kc):
    K, N = w.shape
    return np.ascontiguousarray(w.reshape(kc, 128, N).transpose(1, 0, 2))


def prep_shared(inp):
    f32 = np.float32
    sh = {}
    wg = np.empty((L * 2, NG, 128, 8, G * 128), f32)
    wu = np.empty_like(wg)
    wd = np.empty((L * 2, NG, 128, G, D), f32)
    for l in range(L):
        for which, nm in enumerate(("ffn1", "ffn2")):
            f = l * 2 + which
            g_ = np.asarray(inp[nm + "_w_gate"][l], f32)
            u_ = np.asarray(inp[nm + "_w_up"][l], f32)
            d_ = np.asarray(inp[nm + "_w_down"][l], f32)
            for gi in range(NG):
                cs = slice(gi * G * 128, (gi + 1) * G * 128)
                wg[f, gi] = _wk(g_[:, cs], 8)
                wu[f, gi] = _wk(u_[:, cs], 8)
                wd[f, gi] = d_[cs, :].reshape(G, 128, D).transpose(1, 0, 2)
    sh["wg"], sh["wu"], sh["wd"] = wg, wu, wd
    w_in = np.asarray(inp["w_in"], f32)
    sh["wvg"] = np.stack([np.stack([_wk(w_in[l][:, 1024:1536], 8), _wk(w_in[l][:, 2048:2560], 8)]) for l in range(L)])
    sh["wgu"] = np.stack([_wk(w_in[l][:, 1536:2048], 8) for l in range(L)])
    sh["wqk"] = np.stack([np.stack([_wk(np.concatenate([w_in[l][:, p * 128:(p + 1) * 128],
                                                        w_in[l][:, 512 + p * 128:512 + (p + 1) * 128]], 1), 8)
                                    for p in range(4)]) for l in range(L)])
    sh["wo"] = np.stack([_wk(np.asarray(inp["w_out"][l], f32), 8) for l in range(L)])
    sh["wpg"] = np.stack([np.ascontiguousarray(_wk(np.asarray(inp["ple_w_gate"][l], f32), 8).reshape(128, 8, 8, 128).transpose(2, 0, 1, 3))
                          for l in range(L)])
    sh["wpp"] = np.stack([_wk(np.asarray(inp["ple_w_proj"][l], f32), 2) for l in range(L)])
    ws = np.asarray(inp["gmlp_w_s"], f32)
    sh["wsT"] = np.ascontiguousarray(ws.transpose(0, 3, 1, 2).reshape(L, 128, 8 * 128))
    sh["bs"] = np.ascontiguousarray(np.asarray(inp["gmlp_b_s"], f32).reshape(L, 1, 1024))
    sh["lng"] = np.ascontiguousarray(np.broadcast_to(np.asarray(inp["gmlp_ln_g"], f32)[:, None, :], (L, 128, 512)))
    sh["lnb"] = np.ascontiguousarray(np.broadcast_to(np.asarray(inp["gmlp_ln_b"], f32)[:, None, :], (L, 128, 512)))
    pp = np.zeros((128, L * PL), f32)
    for l in range(L):
        for nm, key in (("ffn1_pre", "ffn1_pre_norm"), ("ffn1_post", "ffn1_post_norm"), ("mix_pre", "mix_pre_norm"),
                        ("mix_post", "mix_post_norm"), ("ffn2_pre", "ffn2_pre_norm"), ("ffn2_post", "ffn2_post_norm"),
                        ("ple_pre", "ple_pre_norm"), ("ple_post", "ple_post_norm")):
            pp[:, pcol(l, nm):pcol(l, nm) + 8] = _fm(inp[key][l], 8)
        pp[:, pcol(l, "attn_out"):pcol(l, "attn_out") + 4] = _fm(inp["attn_out_norm"][l], 4)
        pp[:, pcol(l, "gmlp_out"):pcol(l, "gmlp_out") + 4] = _fm(inp["gmlp_out_norm"][l], 4)
    sh["pp"] = pp
    i = np.arange(128)
    cb = np.zeros((128, 512), f32)
    cb[:, 0:128] = 1.0
    cb[:, 128:256] = np.eye(128, dtype=f32)
    cb[:, 256:384] = np.where(i[:, None] > i[None, :], -BIG, 0.0)
    cb[:, 384:512] = (i[:, None] <= i[None, :]).astype(f32)
    sh["cb"] = cb
    slopes = np.array([0.5 ** (h + 1) for h in range(8)], f32)
    cf = np.zeros((128, 256), f32)
    for h in range(8):
        for d in range(16):
            cf[:, h * 16 + d] = slopes[h] * (i + 128.0 * (d - 12))
    for b in range(8):
        for j in range(8):
            cf[:, 128 + b * 8 + j] = 0.0 if j < b else -1e30
            cf[:, 192 + b * 8 + j] = -BIG if j < b else 0.0
    sh["cf"] = cf
    t = np.arange(S)
    dl = t % 512
    qaug = np.zeros((8, 2, S), f32)
    for h in range(8):
        qaug[h, 0] = -8.0 * slopes[h] * (dl % 256)
        qaug[h, 1] = -8.0 * slopes[h] * 256.0 * (dl // 256)
    sh["qaug"] = qaug
    kaug = np.zeros((10, S), f32)
    for j in range(8):
        kaug[j] = (t // 256 == j)
    kaug[8:10] = 1.0
    sh["kaug"] = kaug
    return sh


def prep_core(inp, core, n_seq=NS):
    x = np.asarray(inp["x"], np.float32)
    p = np.asarray(inp["p"], np.float32)
    b0 = core * n_seq
    xT = np.stack([x[b0 + s].T.reshape(8, 128, S).transpose(1, 0, 2) for s in range(n_seq)])
    pT = np.stack([np.stack([p[l, b0 + s].T.reshape(2, 128, S).transpose(1, 0, 2) for s in range(n_seq)]) for l in range(L)])
    return {"xT": np.ascontiguousarray(xT), "pT": np.ascontiguousarray(pT)}


def kernel(**inputs):
    b = get_builder()
    sh = prep_shared(inputs)
    in_maps = []
    for core in range(N_CORES):
        m = dict(sh)
        m.update(prep_core(inputs, core))
        in_maps.append(m)
    res = run_bass_kernel_spmd(b.nc, in_maps, core_ids=list(range(N_CORES)))
    out = np.empty((16, S, D), np.float32)
    for core in range(N_CORES):
        oT = res.results[core]["outT"]
        for s in range(NS):
            out[core * NS + s] = oT[s].transpose(2, 1, 0).reshape(S, D)
    return out
```
